# Optimizing a Trainium2 kernel written in Bass

```python
import jax, jax.numpy as jnp
from jax import lax
import numpy as np

D_MODEL = 1024
BATCH = 16
SEQ = 2048
DEPTH = 1

GRID_W = 64
CTX_LEN = 256

HGRN_HEADS = 4
HGRN_DK = 128
HGRN_DV = 128
HGRN_W = HGRN_HEADS * HGRN_DK
GLA_HEADS = 4
GLA_DK = 64
GLA_DV = 128
GLA_WK = GLA_HEADS * GLA_DK
GLA_WV = GLA_HEADS * GLA_DV
GLA_GATE_RANK = 16
GLA_GATE_NORMALIZER = 16.0

CHUNK = 64

IN_SIZES = (HGRN_W, HGRN_W, HGRN_W, HGRN_W, HGRN_W,
            GLA_WK, GLA_WK, GLA_WV, GLA_GATE_RANK, GLA_GATE_RANK, GLA_WV,
            D_MODEL, D_MODEL)
IN_DIM = sum(IN_SIZES)

N_EXPERTS = 32
TOP_K = 4
D_EXPERT = 1024
SWIGLU_LIMIT = 7.0
SWIGLU_ALPHA = 1.702
MOE_BLOCK = 256

NORM_EPS = 1e-6

kernel_name = "hybrid_hgrn2_gla_moe_prefix_dit"


def _rms_norm(x, w):
    xf = x.astype(jnp.float32)
    y = xf * lax.rsqrt(jnp.mean(xf * xf, axis=-1, keepdims=True) + NORM_EPS) * w.astype(jnp.float32)
    return y.astype(x.dtype)


def _split_points():
    pts, acc = [], 0
    for s in IN_SIZES[:-1]:
        acc += s
        pts.append(acc)
    return pts


def _heads(t, n_heads):
    b_, l_, w_ = t.shape
    return t.reshape(b_, l_, n_heads, w_ // n_heads).transpose(0, 2, 1, 3)


def _gated_chunk_scan(q, k, v, log_g, s0):
    b_, h_, l_, _ = q.shape
    dv = v.shape[-1]
    n = l_ // CHUNK

    def to_chunks(t):
        return jnp.moveaxis(t.astype(jnp.float32).reshape(b_, h_, n, CHUNK, t.shape[-1]), 2, 0)

    incl = jnp.tril(jnp.ones((CHUNK, CHUNK), dtype=bool))[:, :, None]

    def step(state, inp):
        qc, kc, vc, gc = inp
        cum = jnp.cumsum(gc, axis=2)
        rel = cum[:, :, :, None, :] - cum[:, :, None, :, :]
        decay = jnp.exp(jnp.where(incl, rel, -jnp.inf))
        scores = jnp.einsum('bhid,bhjd,bhijd->bhij', qc, kc, decay)
        o = (jnp.einsum('bhij,bhjv->bhiv', scores, vc)
             + jnp.einsum('bhid,bhdv->bhiv', qc * jnp.exp(cum), state))
        last = cum[:, :, -1:, :]
        k_dec = kc * jnp.exp(last - cum)
        state = jnp.exp(last[:, :, 0, :, None]) * state + jnp.einsum('bhjd,bhjv->bhdv', k_dec, vc)
        return state, o

    s_fin, o = lax.scan(step, s0, (to_chunks(q), to_chunks(k), to_chunks(v), to_chunks(log_g)))
    o = jnp.moveaxis(o, 0, 2).reshape(b_, h_, l_, dv)
    return o, s_fin


def _zero_state(seq):
    q, v = seq[0], seq[3]
    return jnp.zeros((q.shape[0], q.shape[1], q.shape[3], v.shape[3]), jnp.float32)


def _bidir_scan(seq, s0_f, s0_b):
    q, kf, kb, v, gf, gb = seq
    flip = lambda t: jnp.flip(t, axis=2)
    o_f, s_f = _gated_chunk_scan(q, kf, v, gf, s0_f)
    o_b, s_b = _gated_chunk_scan(flip(q), flip(kb), flip(v), flip(gb), s0_b)
    return o_f + flip(o_b), s_f, s_b


def _hgrn2_inputs(q, z_f, z_b, i, lb):
    def gates(z, lb_d):
        z = z.astype(jnp.float32)
        log_f = jnp.log(lb_d + (1.0 - lb_d) * jax.nn.sigmoid(z))
        key = (1.0 - lb_d) * jax.nn.sigmoid(-z)
        return _heads(key, HGRN_HEADS), _heads(log_f, HGRN_HEADS)
    kf, gf = gates(z_f, lb[0])
    kb, gb = gates(z_b, lb[1])
    return (_heads(q, HGRN_HEADS), kf, kb, _heads(i, HGRN_HEADS), gf, gb)


def _gla_inputs(q, k, v, r_f, r_b, gk_w2, gk_b):
    gf = jax.nn.log_sigmoid((r_f @ gk_w2[0] + gk_b[0]).astype(jnp.float32)) / GLA_GATE_NORMALIZER
    gb = jax.nn.log_sigmoid((r_b @ gk_w2[1] + gk_b[1]).astype(jnp.float32)) / GLA_GATE_NORMALIZER
    kh = _heads(k, GLA_HEADS)
    return (_heads(q * (GLA_DK ** -0.5), GLA_HEADS), kh, kh, _heads(v, GLA_HEADS),
            _heads(gf, GLA_HEADS), _heads(gb, GLA_HEADS))


def _head_readout(o, norm_w, gate):
    o = o * lax.rsqrt(jnp.mean(o * o, axis=-1, keepdims=True) + NORM_EPS) * norm_w.astype(jnp.float32)
    b_, h_, l_, dv = o.shape
    o = o.transpose(0, 2, 1, 3).reshape(b_, l_, h_ * dv)
    return (o * jax.nn.silu(gate.astype(jnp.float32))).astype(gate.dtype)


def _hybrid_mixer(h_lat, h_ctx, need_ctx_out, w_in, lb, hgrn_norm_w, gla_gk_w2, gla_gk_b,
                  gla_norm_w, w_up_a, w_up_b, w_o):
    split_at = _split_points()

    def prepare(h):
        (qa, za_f, za_b, ia, oga, qb, kb, vb, rb_f, rb_b, ogb, mga, mgb) = jnp.split(h @ w_in, split_at, axis=-1)
        seq_a = _hgrn2_inputs(qa, za_f, za_b, ia, lb)
        seq_b = _gla_inputs(qb, kb, vb, rb_f, rb_b, gla_gk_w2, gla_gk_b)
        return seq_a, seq_b, (oga, ogb, mga, mgb)

    def merge(o_a, o_b, gates):
        oga, ogb, mga, mgb = gates
        y_a = _head_readout(o_a, hgrn_norm_w, oga) @ w_up_a
        y_b = _head_readout(o_b, gla_norm_w, ogb) @ w_up_b
        return (jax.nn.sigmoid(mga) * y_a + jax.nn.sigmoid(mgb) * y_b) @ w_o

    ctx_a, ctx_b, ctx_gates = prepare(h_ctx)
    oc_a, sa_f, sa_b = _bidir_scan(ctx_a, _zero_state(ctx_a), _zero_state(ctx_a))
    oc_b, sb_f, sb_b = _bidir_scan(ctx_b, _zero_state(ctx_b), _zero_state(ctx_b))
    lat_a, lat_b, lat_gates = prepare(h_lat)
    ol_a, _, _ = _bidir_scan(lat_a, sa_f, sa_b)
    ol_b, _, _ = _bidir_scan(lat_b, sb_f, sb_b)
    out_lat = merge(ol_a, ol_b, lat_gates)
    out_ctx = merge(oc_a, oc_b, ctx_gates) if need_ctx_out else None
    return out_lat, out_ctx


def _moe(xt, w_router, b_router, w_gate, b_gate, w_up, b_up, w_down, b_down):
    t_ = xt.shape[0]
    logits = (xt @ w_router).astype(jnp.float32) + b_router.astype(jnp.float32)
    top_val, top_idx = lax.top_k(logits, TOP_K)
    probs = jax.nn.softmax(top_val, axis=-1)
    n_pairs = t_ * TOP_K
    flat_e = top_idx.reshape(-1)
    flat_tok = jnp.arange(n_pairs, dtype=jnp.int32) // TOP_K
    flat_w = probs.reshape(-1)
    order = jnp.argsort(flat_e)
    e_sorted, tok_sorted, w_sorted = flat_e[order], flat_tok[order], flat_w[order]
    counts = jnp.zeros((N_EXPERTS,), jnp.int32).at[flat_e].add(1)
    padded = (counts + MOE_BLOCK - 1) // MOE_BLOCK * MOE_BLOCK
    padded_end = jnp.cumsum(padded)
    padded_start = padded_end - padded
    start = jnp.cumsum(counts) - counts
    rank = jnp.arange(n_pairs, dtype=jnp.int32) - start[e_sorted]
    dest = padded_start[e_sorted] + rank
    n_blocks = -(-n_pairs // MOE_BLOCK) + N_EXPERTS
    p_len = n_blocks * MOE_BLOCK
    tok_pad = jnp.full((p_len,), t_, jnp.int32).at[dest].set(tok_sorted)
    w_pad = jnp.zeros((p_len,), jnp.float32).at[dest].set(w_sorted)
    block_expert = jnp.minimum(
        jnp.searchsorted(padded_end, jnp.arange(n_blocks, dtype=jnp.int32) * MOE_BLOCK, side='right'),
        N_EXPERTS - 1)
    xt_pad = jnp.concatenate([xt, jnp.zeros((1, xt.shape[1]), xt.dtype)], axis=0)

    def expert_block(args):
        tok_b, e = args
        xb = xt_pad[tok_b]
        g = jnp.minimum(xb @ w_gate[e] + b_gate[e], SWIGLU_LIMIT)
        u = jnp.clip(xb @ w_up[e] + b_up[e], -SWIGLU_LIMIT, SWIGLU_LIMIT)
        act = g * jax.nn.sigmoid(SWIGLU_ALPHA * g) * (u + 1.0)
        return act @ w_down[e] + b_down[e]

    ys = lax.map(expert_block, (tok_pad.reshape(n_blocks, MOE_BLOCK), block_expert))
    ys = ys.reshape(p_len, -1) * w_pad[:, None].astype(ys.dtype)
    return jax.ops.segment_sum(ys, tok_pad, num_segments=t_ + 1)[:t_]


def setup_inputs(seed: int = 0) -> dict:
    key = jax.random.key(seed)
    ks = jax.random.split(key, 27)
    nrm = lambda k, shape, s: jax.random.normal(k, shape, jnp.float32) * s
    gain = lambda k, shape: 1.0 + 0.02 * jax.random.normal(k, shape, jnp.float32)
    D, E, F = D_MODEL, N_EXPERTS, D_EXPERT
    return {
        "x": nrm(ks[0], (BATCH, SEQ, D), 1.0),
        "c": nrm(ks[1], (BATCH, D), 1.0),
        "ctx": nrm(ks[2], (BATCH, CTX_LEN, D), 1.0),
        "c_ctx": nrm(ks[3], (D,), 1.0),
        "w_ada": nrm(ks[4], (DEPTH, D, 6 * D), 0.5 * D ** -0.5),
        "b_ada": nrm(ks[5], (DEPTH, 6 * D), 0.01),
        "g_pre_mix": gain(ks[6], (DEPTH, D)),
        "g_post_mix": gain(ks[7], (DEPTH, D)),
        "g_pre_ffn": gain(ks[8], (DEPTH, D)),
        "g_post_ffn": gain(ks[9], (DEPTH, D)),
        "w_in": nrm(ks[10], (DEPTH, D, IN_DIM), D ** -0.5),
        "hgrn_lb": nrm(ks[11], (DEPTH + 1, 2, HGRN_W), 0.1),
        "hgrn_norm_w": gain(ks[12], (DEPTH, HGRN_DV)),
        "gla_gk_w2": nrm(ks[13], (DEPTH, 2, GLA_GATE_RANK, GLA_WK), GLA_GATE_RANK ** -0.5),
        "gla_gk_b": nrm(ks[14], (DEPTH, 2, GLA_WK), 0.1),
        "gla_norm_w": gain(ks[15], (DEPTH, GLA_DV)),
        "w_up_a": nrm(ks[16], (DEPTH, HGRN_W, D), HGRN_W ** -0.5),
        "w_up_b": nrm(ks[17], (DEPTH, GLA_WV, D), GLA_WV ** -0.5),
        "w_o": nrm(ks[18], (DEPTH, D, D), D ** -0.5),
        "w_router": nrm(ks[19], (DEPTH, D, E), D ** -0.5),
        "b_router": nrm(ks[20], (DEPTH, E), 0.01),
        "w_gate": nrm(ks[21], (DEPTH, E, D, F), D ** -0.5),
        "b_gate": nrm(ks[22], (DEPTH, E, F), 0.01),
        "w_up": nrm(ks[23], (DEPTH, E, D, F), D ** -0.5),
        "b_up": nrm(ks[24], (DEPTH, E, F), 0.01),
        "w_down": nrm(ks[25], (DEPTH, E, F, D), F ** -0.5),
        "b_down": nrm(ks[26], (DEPTH, E, D), 0.01),
    }


def reference(x, c, ctx, c_ctx, w_ada, b_ada, g_pre_mix, g_post_mix, g_pre_ffn, g_post_ffn,
              w_in, hgrn_lb, hgrn_norm_w, gla_gk_w2, gla_gk_b, gla_norm_w, w_up_a, w_up_b, w_o,
              w_router, b_router, w_gate, b_gate, w_up, b_up, w_down, b_down):
    lb_all = jnp.cumsum(jax.nn.softmax(hgrn_lb.astype(jnp.float32), axis=0), axis=0)
    b_, l_, d_ = x.shape
    for layer in range(DEPTH):
        last = layer == DEPTH - 1
        mod = jax.nn.silu(c) @ w_ada[layer] + b_ada[layer]
        sh1, sc1, gt1, sh2, sc2, gt2 = jnp.split(mod[:, None, :], 6, axis=-1)
        cmod = jnp.split(jax.nn.silu(c_ctx) @ w_ada[layer] + b_ada[layer], 6)

        h = _rms_norm(x, g_pre_mix[layer]) * (1.0 + sc1) + sh1
        hc = _rms_norm(ctx, g_pre_mix[layer]) * (1.0 + cmod[1]) + cmod[0]
        mix_l, mix_c = _hybrid_mixer(h, hc, not last, w_in[layer], lb_all[layer], hgrn_norm_w[layer],
                                     gla_gk_w2[layer], gla_gk_b[layer], gla_norm_w[layer],
                                     w_up_a[layer], w_up_b[layer], w_o[layer])
        x = x + gt1 * _rms_norm(mix_l, g_post_mix[layer])

        h2 = _rms_norm(x, g_pre_ffn[layer]) * (1.0 + sc2) + sh2
        ffn = _moe(h2.reshape(b_ * l_, d_), w_router[layer], b_router[layer], w_gate[layer], b_gate[layer],
                   w_up[layer], b_up[layer], w_down[layer], b_down[layer]).reshape(b_, l_, d_)
        x = x + gt2 * _rms_norm(ffn, g_post_ffn[layer])

        if not last:
            ctx = ctx + cmod[2] * _rms_norm(mix_c, g_post_mix[layer])
            hc2 = _rms_norm(ctx, g_pre_ffn[layer]) * (1.0 + cmod[4]) + cmod[3]
            lc = ctx.shape[1]
            ffn_c = _moe(hc2.reshape(b_ * lc, d_), w_router[layer], b_router[layer], w_gate[layer], b_gate[layer],
                         w_up[layer], b_up[layer], w_down[layer], b_down[layer]).reshape(b_, lc, d_)
            ctx = ctx + cmod[5] * _rms_norm(ffn_c, g_post_ffn[layer])
    return x
```

```python
import numpy as np
from contextlib import ExitStack
import concourse.bass as bass
import concourse.mybir as mybir
from concourse.bass_utils import run_bass_kernel_spmd

F32 = mybir.dt.float32
BF16 = mybir.dt.bfloat16
U8 = mybir.dt.uint8
I32 = mybir.dt.int32
U32 = mybir.dt.uint32
AF = mybir.ActivationFunctionType
ALU = mybir.AluOpType

D = 1024
NSEQ = 2
LAT = 2048
CTXL = 256
ST = 256
INW = 6176
EPS = 1e-6
NEXP = 32

ENGS = ("pe", "act", "dve", "pool", "sp")
EPOCH = 16384
ND = 8


class Buf:
    __slots__ = ("name", "last_w", "readers")

    def __init__(self, name):
        self.name = name
        self.last_w = None
        self.readers = []


class Op:
    __slots__ = ("eng", "fn", "deps", "dma", "done", "gid", "inc", "guard")

    def __init__(self, eng, fn, dma):
        self.eng = eng
        self.fn = fn
        self.dma = dma
        self.deps = set()
        self.done = None
        self.inc = 0
        self.guard = None


class Sched:
    def __init__(self):
        self.ops = {e: [] for e in ENGS}
        self.n = 0
        self.fence_ops = set()
        self.cnt_regs = {}
        self.ext_cache = {}
        self.thr_regs = {}
        self.gran = 256

    def fence(self):
        f = set()
        for e in ENGS:
            comp = [o for o in self.ops[e] if not o.dma]
            if comp:
                f.add(comp[-1])
            dm = [o for o in self.ops[e] if o.dma]
            f.update(dm[-ND:])
        self.fence_ops = f

    def op(self, eng, fn, r=(), w=(), dma=False, guard=None):
        o = Op(eng, fn, dma)
        o.guard = guard
        o.gid = self.n
        self.n += 1
        for b in r:
            if b.last_w is not None:
                o.deps.add(b.last_w)
        for b in w:
            if b.last_w is not None:
                o.deps.add(b.last_w)
            for rd in b.readers:
                o.deps.add(rd)
        for b in r:
            b.readers.append(o)
        for b in w:
            b.last_w = o
            b.readers = []
        o.deps.update(self.fence_ops)
        o.deps.discard(o)
        self.ops[eng].append(o)
        return o

    def finalize(self, nc, ctx):
        self.sems = {}
        self.groups = {}
        for e in ENGS:
            for o in self.ops[e]:
                if o.guard is not None:
                    self.groups.setdefault(o.guard, []).append(o)
        for e in ENGS:
            kc = 0
            kd = 0
            dq = []
            for o in self.ops[e]:
                if o.dma:
                    slot = kd % ND
                    key = (e, "d", slot)
                    if key not in self.sems:
                        self.sems[key] = ctx.enter_context(nc.semaphore("sd_%s_%d" % (e, slot)))
                    o.done = (self.sems[key], 16 * (kd // ND + 1))
                    if kd >= ND:
                        o.deps.add(dq[kd - ND])
                    dq.append(o)
                    kd += 1
                else:
                    ep = kc // EPOCH
                    key = (e, "c", ep)
                    if key not in self.sems:
                        self.sems[key] = ctx.enter_context(nc.semaphore("sc_%s_%d" % (e, ep)))
                    o.done = (self.sems[key], kc % EPOCH + 1)
                    kc += 1

    def emit(self, eng_name, eng):
        known = {}

        def emit_one(o, known):
            for d in sorted(o.deps, key=lambda x: -x.gid):
                sm, v = d.done
                k = id(sm)
                if known.get(k, 0) >= v:
                    continue
                eng.wait_ge(sm, v)
                known[k] = v
            ins = o.fn(eng)
            sm, v = o.done
            ins.then_inc(sm, 16 if o.dma else 1)

        def thr_reg(g):
            key = (eng_name, g)
            if key not in self.thr_regs:
                rg = eng.alloc_register("thr_%s_%d" % (eng_name, g))
                eng.reg_mov(rg, g * self.gran)
                self.thr_regs[key] = rg
            return self.thr_regs[key]

        def emit_chain(chain, known):
            gd, block = chain[0]
            rest = chain[1:]
            thr = thr_reg(gd[1])
            cnt = self.cnt_regs[eng_name]
            with eng.If_lt(thr, cnt):
                kn = dict(known)
                for b in block:
                    emit_one(b, kn)
                if rest:
                    emit_chain(rest, kn)
            with eng.Else():
                gset = frozenset(g_ for g_, _ in chain)
                ck = (gset, )
                if ck not in self.ext_cache:
                    ext = {}
                    for g_ in gset:
                        for b in self.groups[g_]:
                            for d in b.deps:
                                if d.guard in gset:
                                    continue
                                sm, v = d.done
                                if ext.get(id(sm), (None, 0))[1] < v:
                                    ext[id(sm)] = (sm, v)
                    self.ext_cache[ck] = ext
                ext = self.ext_cache[ck]
                allb = [b for _, blk in chain for b in blk]
                if any(not b.dma for b in allb):
                    eng.drain()
                for k_, (sm, v) in ext.items():
                    if known.get(k_, 0) >= v:
                        continue
                    eng.wait_ge(sm, v)
                incs = {}
                for b in allb:
                    sm = b.done[0]
                    n_ = incs.get(id(sm), (sm, 0))[1]
                    incs[id(sm)] = (sm, n_ + (16 if b.dma else 1))
                for k_, (sm, n_) in incs.items():
                    eng.sem_inc(sm, n_)

        ops = self.ops[eng_name]
        for g_ in sorted({o.guard[1] for o in ops if o.guard is not None}):
            thr_reg(g_)
        i = 0
        while i < len(ops):
            o = ops[i]
            if o.guard is None:
                emit_one(o, known)
                i += 1
                continue
            chain = []
            j = i
            seen = set()
            while j < len(ops) and ops[j].guard is not None and ops[j].guard[0] == o.guard[0]:
                gd = ops[j].guard
                assert gd not in seen, ("guard re-appears non-contiguously", gd)
                assert not chain or gd[1] > chain[-1][0][1]
                seen.add(gd)
                k = j
                while k < len(ops) and ops[k].guard == gd:
                    k += 1
                chain.append((gd, ops[j:k]))
                j = k
            emit_chain(chain, known)
            i = j


class Arena:
    def __init__(self, ap, nbytes):
        self.ap = ap
        self.nbytes = nbytes
        self.off = 0
        self.hi = 0

    def alloc(self, shape, dtype, parts=128):
        esz = {F32: 4, BF16: 2, U8: 1, I32: 4, U32: 4}[dtype]
        n = int(np.prod(shape))
        nb = (n * esz + 63) // 64 * 64
        assert self.off + nb <= self.nbytes, ("arena overflow", self.off, nb, self.nbytes)
        v = self.ap[0:parts, self.off:self.off + n * esz]
        if dtype != U8:
            v = v.bitcast(dtype)
        if len(shape) == 2:
            v = v.rearrange("p (a b) -> p a b", a=shape[0])
        elif len(shape) == 3:
            v = v.rearrange("p (a b c) -> p a b c", a=shape[0], b=shape[1])
        self.off += nb
        self.hi = max(self.hi, self.off)
        return v

    def mark(self):
        return self.off

    def release(self, m):
        self.off = m


V_BADA = 0
V_GPRE = 48
V_LBR = 72
V_HNW = 88
V_GNW = 89
V_GKB = 90
V_BG = 94
V_BU = 350
NV = 606


def build_program(dbg=None):
    dbg = dbg or {}
    nc = bass.Bass("TRN2", target_bir_lowering=False)
    dt = nc.dram_tensor
    x_d = dt("x", [NSEQ * LAT, D], F32, kind="ExternalInput").ap()
    ctx_d = dt("ctx", [NSEQ * CTXL, D], F32, kind="ExternalInput").ap()
    cT_d = dt("cT", [128, 8, 3], F32, kind="ExternalInput").ap()
    vecs_d = dt("vecs", [128, NV], F32, kind="ExternalInput").ap()
    rowv_d = dt("rowv", [8, D], F32, kind="ExternalInput").ap()
    w_ada_d = dt("w_ada", [D, 6 * D], F32, kind="ExternalInput").ap()
    w_in_d = dt("w_in", [D, INW], F32, kind="ExternalInput").ap()
    w2_d = dt("gk_w2", [2, 16, 256], F32, kind="ExternalInput").ap()
    wupo_d = dt("wupo", [2048, D], F32, kind="ExternalInput").ap()
    wr_d = dt("w_router", [D, NEXP], F32, kind="ExternalInput").ap()
    wg_d = dt("w_gate", [NEXP, D, D], F32, kind="ExternalInput").ap()
    wu_d = dt("w_up", [NEXP, D, D], F32, kind="ExternalInput").ap()
    wd_d = dt("w_down", [NEXP, D, D], F32, kind="ExternalInput").ap()
    bd_d = dt("b_down", [NEXP, D], F32, kind="ExternalInput").ap()
    out_d = dt("out", [NSEQ * LAT, D], F32, kind="ExternalOutput").ap()
    winb_d = dt("winb", [D, INW], BF16, kind="Internal").ap()
    wupob_d = dt("wupob", [2048, D], BF16, kind="Internal").ap()
    bc_d = dt("bcrows", [NSEQ, 4, D], F32, kind="Internal").ap()
    x1_d = dt("x1s", [NSEQ * LAT, D], F32, kind="Internal").ap()
    dbg_out = {}
    for name, (shape, dtype) in dbg.get("outs", {}).items():
        dbg_out[name] = dt(name, list(shape), dtype, kind="ExternalOutput").ap()

    S = Sched()
    with ExitStack() as ctx:
        arena_t = ctx.enter_context(nc.sbuf_tensor("arena", [128, 206 * 1024], U8))
        A = Arena(arena_t, 206 * 1024)
        banks = [ctx.enter_context(nc.psum_tensor("pb%d" % i, [128, 512], F32)) for i in range(8)]
        PB = [Buf("pb%d" % i) for i in range(8)]

        def T(shape, dtype, name):
            return A.alloc(shape, dtype), Buf(name)

        identb, b_identb = T([128], BF16, "identb")
        identf, b_identf = T([128], F32, "identf")
        maskF, b_maskF = T([4, 128], BF16, "maskF")
        maskB, b_maskB = T([4, 128], BF16, "maskB")
        onesb, b_onesb = T([128], BF16, "onesb")
        onesf, b_onesf = T([128], F32, "onesf")
        m01, b_m01 = T([8, 128], F32, "m01")
        neghalf, b_neghalf = T([8], F32, "neghalf")
        vecs, b_vecs = T([NV], F32, "vecs")
        lbt, b_lbt = T([16], F32, "lbt")
        negb, b_negb = T([4], F32, "negb")
        w2s, b_w2s = T([2, 256], F32, "w2s")
        scT, b_scT = T([8, 3], F32, "scT")
        modT, b_modT = T([48, 3], F32, "modT")
        A1, b_A1 = T([3, 8], F32, "A1")
        tmpf, b_tmpf = T([128], F32, "tmpf")

        def c_memset(eng, ap, val, bufs):
            S.op(eng, lambda e: e.memset(ap, val), w=bufs)

        c_memset("pool", tmpf, 0.0, [b_tmpf])
        S.op("pool", lambda e: e.affine_select(out=identf, in_=tmpf, pattern=[[-1, 128]],
                                               compare_op=ALU.not_equal, fill=1.0, base=0,
                                               channel_multiplier=1), r=[b_tmpf], w=[b_identf])
        S.op("dve", lambda e: e.tensor_copy(out=identb, in_=identf), r=[b_identf], w=[b_identb])
        c_memset("pool", onesf, 1.0, [b_onesf])
        c_memset("pool", onesb, 1.0, [b_onesb])
        c_memset("pool", neghalf, -0.5, [b_neghalf])
        c_memset("pool", m01, 1.0, [b_m01])
        c_memset("pool", m01[:, :, 0:1], 0.0, [b_m01])
        S.op("sp", lambda e: e.dma_start(out=vecs, in_=vecs_d), w=[b_vecs], dma=True)
        S.op("sp", lambda e: e.dma_start(out=scT, in_=cT_d), w=[b_scT], dma=True)
        S.op("sp", lambda e: e.dma_start(out=w2s[0:16], in_=w2_d.rearrange("d r c -> r d c")),
             w=[b_w2s], dma=True)
        S.op("dve", lambda e: e.tensor_tensor(out=lbt[:, 0:8], in0=vecs[:, V_LBR:V_LBR + 8],
                                              in1=vecs[:, V_LBR + 8:V_LBR + 16], op=ALU.subtract),
             r=[b_vecs], w=[b_lbt])
        S.op("act", lambda e: e.activation(out=lbt[:, 0:8], in_=lbt[:, 0:8], func=AF.Sigmoid),
             r=[b_lbt], w=[b_lbt])
        S.op("dve", lambda e: e.tensor_scalar(out=lbt[:, 8:16], in0=lbt[:, 0:8], scalar1=-1.0, scalar2=1.0,
                                              op0=ALU.mult, op1=ALU.add), r=[b_lbt], w=[b_lbt])
        S.op("dve", lambda e: e.tensor_scalar(out=negb, in0=vecs[:, V_GKB:V_GKB + 4], scalar1=-1.0,
                                              scalar2=None, op0=ALU.mult), r=[b_vecs], w=[b_negb])
        S.op("act", lambda e: e.activation(out=scT, in_=scT, func=AF.Silu), r=[b_scT], w=[b_scT])

        b_winb = Buf("winb")
        b_wupob = Buf("wupob")
        for i in range(4):
            c0 = i * 1544
            S.op("pool", lambda e, c0=c0: e.dma_start(out=winb_d[:, c0:c0 + 1544], in_=w_in_d[:, c0:c0 + 1544]),
                 w=[b_winb], dma=True)
        S.op("pool", lambda e: e.dma_start(out=wupob_d, in_=wupo_d), w=[b_wupob], dma=True)

        m0 = A.mark()
        wab = [T([8, 512], F32, "wab%d" % i) for i in range(2)]
        rep = [T([8, 128], F32, "rep%d" % j) for j in range(NSEQ)]
        bct, b_bct = T([512], F32, "bct")
        bcs, b_bcs = T([512], F32, "bcs")
        ones4, b_ones4 = T([4, 128], F32, "ones4")
        c_memset("pool", ones4, 1.0, [b_ones4])
        mtmp, b_mtmp = T([4, 128], F32, "mtmp")
        S.op("pool", lambda e: e.affine_select(out=mtmp, in_=ones4, pattern=[[0, 4], [1, 128]],
                                               compare_op=ALU.is_ge, fill=0.0, base=0,
                                               channel_multiplier=-1), r=[b_ones4], w=[b_mtmp])
        S.op("dve", lambda e: e.tensor_copy(out=maskF, in_=mtmp), r=[b_mtmp], w=[b_maskF])
        S.op("pool", lambda e: e.affine_select(out=mtmp, in_=ones4, pattern=[[0, 4], [-1, 128]],
                                               compare_op=ALU.is_ge, fill=0.0, base=0,
                                               channel_multiplier=1), r=[b_ones4], w=[b_mtmp])
        S.op("dve", lambda e: e.tensor_copy(out=maskB, in_=mtmp), r=[b_mtmp], w=[b_maskB])

        for j in range(NSEQ):
            for kc in range(8):
                S.op("act", lambda e, j=j, kc=kc: e.activation(out=rep[j][0][:, kc, :], in_=onesf, func=AF.Identity,
                                                               scale=scT[:, kc, j:j + 1]),
                     r=[b_onesf, b_scT], w=[rep[j][1]])
        b_bcd = Buf("bc_d")
        bcseg = {4: 0, 5: 0, 6: 1, 7: 1, 8: 2, 9: 2, 10: 3, 11: 3}
        for blk in range(12):
            wt, wb_ = wab[blk % 2]
            S.op("sp", lambda e, wt=wt, blk=blk: e.dma_start(
                out=wt, in_=w_ada_d[:, blk * 512:(blk + 1) * 512].rearrange("(k p) n -> p k n", p=128)),
                w=[wb_], dma=True)
            def fm(e, wt=wt, blk=blk):
                ins = None
                for m in range(4):
                    for kc in range(8):
                        ins = e.matmul(banks[0][:, (blk * 4 + m) * 3:(blk * 4 + m) * 3 + 3],
                                       lhsT=wt[:, kc, m * 128:(m + 1) * 128], rhs=scT[:, kc, :],
                                       start=(kc == 0), stop=(kc == 7))
                return ins
            S.op("pe", fm, r=[wb_, b_scT], w=[PB[0]])
            if blk in bcseg:
                for j in range(NSEQ):
                    pbk = 1 + j
                    def bc(e, wt=wt, j=j, pbk=pbk):
                        ins = None
                        for kc in range(8):
                            ins = e.matmul(banks[pbk][:, :], lhsT=rep[j][0][:, kc, :], rhs=wt[:, kc, :],
                                           start=(kc == 0), stop=(kc == 7))
                        return ins
                    S.op("pe", bc, r=[wb_, rep[j][1]], w=[PB[pbk]])
                    row = bcseg[blk]
                    half = blk % 2
                    S.op("sp", lambda e, row=row, half=half: e.dma_start(
                        out=bct, in_=rowv_d[row:row + 1, half * 512:(half + 1) * 512].to_broadcast([128, 512])),
                        w=[b_bct], dma=True)
                    S.op("dve", lambda e, pbk=pbk: e.tensor_tensor(out=bcs, in0=banks[pbk][:, :], in1=bct, op=ALU.add),
                         r=[PB[pbk], b_bct], w=[b_bcs])
                    if row == 2:
                        S.op("dve", lambda e: e.tensor_scalar(out=bcs, in0=bcs, scalar1=1.0, scalar2=None, op0=ALU.add),
                             r=[b_bcs], w=[b_bcs])
                    grow = {0: 4, 2: 5, 3: 6}.get(row)
                    if grow is not None:
                        S.op("sp", lambda e, grow=grow, half=half: e.dma_start(
                            out=bct, in_=rowv_d[grow:grow + 1, half * 512:(half + 1) * 512].to_broadcast([128, 512])),
                            w=[b_bct], dma=True)
                        S.op("dve", lambda e: e.tensor_tensor(out=bcs, in0=bcs, in1=bct, op=ALU.mult),
                             r=[b_bcs, b_bct], w=[b_bcs])
                    S.op("sp", lambda e, j=j, row=row, half=half: e.dma_start(
                        out=bc_d[j, row:row + 1, half * 512:(half + 1) * 512], in_=bcs[0:1, :]),
                        r=[b_bcs], w=[b_bcd], dma=True)
        S.op("dve", lambda e: e.tensor_tensor(
            out=modT, in0=banks[0][:, 0:144].rearrange("p (c j) -> p c j", j=3),
            in1=vecs[:, V_BADA:V_BADA + 48].unsqueeze(2).to_broadcast([128, 48, 3]), op=ALU.add),
            r=[PB[0], b_vecs], w=[b_modT])
        for j in range(3):
            S.op("dve", lambda e, j=j: e.scalar_tensor_tensor(
                out=A1[:, j, :], in0=modT[:, 8:16, j], scalar=1.0, in1=vecs[:, V_GPRE:V_GPRE + 8],
                op0=ALU.add, op1=ALU.mult), r=[b_modT, b_vecs], w=[b_A1])
        A.release(m0)
        S.fence()

        if "modT" in dbg_out:
            S.op("sp", lambda e: e.dma_start(out=dbg_out["modT"], in_=modT), r=[b_modT], w=[Buf("o")], dma=True)
        if "A1" in dbg_out:
            S.op("sp", lambda e: e.dma_start(out=dbg_out["A1"], in_=A1), r=[b_A1], w=[Buf("o")], dma=True)
        if "lbt" in dbg_out:
            S.op("sp", lambda e: e.dma_start(out=dbg_out["lbt"], in_=lbt), r=[b_lbt], w=[Buf("o")], dma=True)
        if "bc" in dbg_out:
            S.op("sp", lambda e: e.dma_start(out=dbg_out["bc"], in_=bc_d), r=[b_bcd], w=[Buf("o")], dma=True)

        def dump(name, ap, bufs):
            if name in dbg_out:
                S.op("sp", lambda e: e.dma_start(out=dbg_out[name], in_=ap), r=bufs, w=[Buf("o")], dma=True)

        CAP = dbg.get("cap", 2048)
        GR = 256
        NGR = CAP // GR
        NROWS = NEXP * CAP
        xg_d = dt("xg", [NROWS, D], BF16, kind="Internal").ap()
        yg_d = dt("yg", [NROWS, D], F32, kind="Internal").ap()
        b_xg = Buf("xg")
        b_yg = Buf("yg")
        NTT = NSEQ * LAT // 128
        P_all, b_Pall = T([NTT, NEXP], F32, "P_all")
        pk_all, b_pkall = T([NTT, 4], F32, "pk_all")
        desti, b_desti = T([NTT, 4], I32, "desti")
        basec, b_basec = T([NEXP], F32, "basec")
        cnti, b_cnti = T([NEXP], I32, "cnti")
        ecap, b_ecap = T([NEXP], F32, "ecap")
        ecapi, b_ecapi = T([NEXP], I32, "ecapi")
        ustr, b_ustr = T([128], BF16, "ustr")
        brt, b_brt = T([NEXP], F32, "brt")
        wr, b_wr = T([8, NEXP], F32, "wr")
        S.op("pool", lambda e: e.memset(basec, 0.0), w=[b_basec])
        S.op("pool", lambda e: e.iota(ecapi, pattern=[[CAP, NEXP]], base=0, channel_multiplier=0), w=[b_ecapi])
        S.op("dve", lambda e: e.tensor_copy(out=ecap, in_=ecapi), r=[b_ecapi], w=[b_ecap])
        S.op("pool", lambda e: e.affine_select(out=tmpf, in_=onesf, pattern=[[1, 128]], compare_op=ALU.is_gt, fill=0.0,
                                               base=0, channel_multiplier=-1), r=[b_onesf], w=[b_tmpf])
        S.op("dve", lambda e: e.tensor_copy(out=ustr, in_=tmpf), r=[b_tmpf], w=[b_ustr])
        S.op("sp", lambda e: e.dma_start(out=brt, in_=rowv_d[7:8, 0:NEXP].to_broadcast([128, NEXP])), w=[b_brt], dma=True)
        S.op("sp", lambda e: e.dma_start(out=wr, in_=wr_d.rearrange("(k p) n -> p k n", p=128)), w=[b_wr], dma=True)
        mix_mark = A.mark()
        NW = 3
        wbuf = [T([8, 512], BF16, "wbuf%d" % i) for i in range(NW)]
        wctr = [0]

        def wload(src_ap, view=None):
            wt, wb_ = wbuf[wctr[0] % NW]
            wctr[0] += 1
            dst = view(wt) if view is not None else wt
            S.op("sp", lambda e: e.dma_start(out=dst, in_=src_ap), r=[b_winb, b_wupob], w=[wb_], dma=True)
            return wt, wb_

        pjc = [0]

        def pjbank():
            i = 1 + (pjc[0] % 3)
            pjc[0] += 1
            return banks[i], PB[i]

        xt = [T([1024], F32, "xt%d" % i) for i in range(2)]
        xnb = [T([1024], BF16, "xnb%d" % i) for i in range(2)]
        hT, b_hT = T([8, ST], BF16, "hT")
        ss, b_ss = T([8], F32, "ss")
        b_ssr = Buf("ssr")
        b_st2r = Buf("st2r")
        junk, b_junk = T([1024], BF16, "junk")
        qA, b_qA = T([4, ST], BF16, "qA")
        fA, b_fA = T([4, ST], F32, "fA")
        lfA, b_lfA = T([4, ST], F32, "lfA")
        preA, b_preA = T([4, ST], F32, "preA")
        EpA, b_EpA = T([4, ST], BF16, "EpA")
        EmA, b_EmA = T([4, ST], BF16, "EmA")
        qeA = [T([4, ST], BF16, "qeA%d" % d) for d in range(2)]
        keA = [T([4, ST], BF16, "keA%d" % d) for d in range(2)]
        aA = [T([4, 2], F32, "aA%d" % d) for d in range(2)]
        rT, b_rT = T([2, ST], F32, "rT")
        qB, b_qB = T([2, ST], BF16, "qB")
        kB, b_kB = T([2, ST], BF16, "kB")
        sB, b_sB = T([2, ST], F32, "sB")
        preB, b_preB = T([2, ST], F32, "preB")
        EpB, b_EpB = T([2, ST], BF16, "EpB")
        EmB, b_EmB = T([2, ST], BF16, "EmB")
        qeB = [T([2, 2, ST], BF16, "qeB%d" % d) for d in range(2)]
        pm, b_pm = T([2], F32, "pm")
        S.op("pool", lambda e: e.memset(pm, 0.0), w=[b_pm])
        S.op("pool", lambda e: e.memset(pm[0:64, 0:1], 1.0), w=[b_pm])
        S.op("pool", lambda e: e.memset(pm[64:128, 1:2], 1.0), w=[b_pm])
        keB = [T([2, ST], BF16, "keB%d" % d) for d in range(2)]
        aB = [T([2, 2], F32, "aB%d" % d) for d in range(2)]
        vA, b_vA = T([2, 512], BF16, "vA")
        vB, b_vB = T([2, 512], BF16, "vB")
        sgA, b_sgA = T([4, ST], BF16, "sgA")
        sgB, b_sgB = T([4, ST], BF16, "sgB")
        smA, b_smA = T([8, ST], BF16, "smA")
        smB, b_smB = T([8, ST], BF16, "smB")
        kdA, b_kdA = T([4, 128], BF16, "kdA")
        kdB, b_kdB = T([2, 128], BF16, "kdB")
        WA, b_WA = T([4, 128], F32, "WA")
        WB, b_WB = T([2, 128], F32, "WB")
        VA, b_VA = T([4, 128], F32, "VA")
        VB, b_VB = T([2, 128], F32, "VB")
        tsA, b_tsA = T([4, 128], F32, "tsA")
        tsB, b_tsB = T([2, 128], F32, "tsB")
        SfA, b_SfA = T([16, 4, 128], BF16, "SfA")
        SfB, b_SfB = T([16, 2, 128], BF16, "SfB")
        SbA, b_SbA = T([4, 128], BF16, "SbA")
        SbB, b_SbB = T([2, 128], BF16, "SbB")
        scF, b_scF = T([4, 128], BF16, "scF")
        scBt, b_scBt = T([4, 128], BF16, "scBt")
        oTs, b_oTs = T([4, 128], F32, "oTs")
        sq, b_sq = T([4, 128], BF16, "sq")
        rstd, b_rstd = T([4, 128], F32, "rstd")
        rdA, b_rdA = T([4, ST], BF16, "rdA")
        rdB, b_rdB = T([4, ST], BF16, "rdB")
        mrgT, b_mrgT = T([8, ST], BF16, "mrgT")
        mt1, b_mt1 = T([ST], F32, "mt1")
        mt2, b_mt2 = T([ST], F32, "mt2")
        mixs, b_mixs = T([1024], F32, "mixs")
        xres, b_xres = T([1024], F32, "xres")
        x1t, b_x1t = T([1024], F32, "x1t")
        G1, b_G1 = T([1024], F32, "G1")
        st2, b_st2 = T([8], F32, "st2")
        A2, b_A2 = T([1024], F32, "A2")
        SH2, b_SH2 = T([1024], F32, "SH2")
        h2b, b_h2b = T([1024], BF16, "h2b")
        lg, b_lg = T([NEXP], F32, "lg")
        mx8, b_mx8 = T([8], F32, "mx8")
        msk, b_msk = T([NEXP], F32, "msk")
        mskb, b_mskb = T([NEXP], BF16, "mskb")
        exq, b_exq = T([NEXP], F32, "exq")
        slot, b_slot = T([NEXP], F32, "slot")
        oh, b_oh = T([NEXP], F32, "oh")
        j32, b_j32 = T([NEXP], F32, "j32")
        destf, b_destf = T([4], F32, "destf")
        sm1, b_sm1 = T([4], F32, "sm1")

        breg = {}

        def bcreg(e):
            if "r" not in breg:
                breg["r"] = e.to_reg(NROWS - 1)
            return breg["r"]

        def m1_tile(gt):
            h2T = xres.rearrange("p (k t) -> p k t", k=8)
            S.op("act", lambda e: e.activation(out=junk, in_=x1t, func=AF.Square, accum_out=st2[:, 5:6]),
                 r=[b_x1t], w=[b_junk, b_st2r])
            S.op("act", lambda e: e.activation(out=st2[:, 1:2], in_=st2[:, 5:6], func=AF.Copy),
                 r=[b_st2r], w=[b_st2])
            S.op("pool", lambda e: e.tensor_scalar(out=st2[:, 1:2], in0=st2[:, 1:2], scalar1=1.0 / D, scalar2=EPS,
                                                   op0=ALU.mult, op1=ALU.add), r=[b_st2], w=[b_st2])
            S.op("pool", lambda e: e.tensor_tensor(out=st2[:, 1:2], in0=st2[:, 1:2], in1=neghalf[:, 0:1], op=ALU.pow),
                 r=[b_st2, b_neghalf], w=[b_st2])
            S.op("dve", lambda e: e.scalar_tensor_tensor(out=mixs, in0=x1t, scalar=st2[:, 1:2], in1=A2,
                                                         op0=ALU.mult, op1=ALU.mult),
                 r=[b_x1t, b_st2, b_A2], w=[b_mixs])
            S.op("dve", lambda e: e.tensor_tensor(out=mixs, in0=mixs, in1=SH2, op=ALU.add),
                 r=[b_mixs, b_SH2], w=[b_mixs])
            S.op("act", lambda e: e.activation(out=h2b, in_=mixs, func=AF.Copy), r=[b_mixs], w=[b_h2b])
            dump("h2b_%d" % gt, h2b, [b_h2b])
            for hf in range(2):
                bk, bb = pjbank()

                def tr(e, hf=hf, bk=bk):
                    ins = None
                    for q in range(4):
                        kc = hf * 4 + q
                        ins = e.transpose(out=bk[:, q * 128:(q + 1) * 128], in_=mixs[:, kc * 128:(kc + 1) * 128],
                                          identity=identf)
                    return ins
                S.op("pe", tr, r=[b_mixs, b_identf], w=[bb])
                S.op("act", lambda e, hf=hf, bk=bk: e.activation(
                    out=xres[:, hf * 512:(hf + 1) * 512], in_=bk[:, :], func=AF.Copy), r=[bb], w=[b_xres])
            bk, bb = pjbank()

            def lgm(e, bk=bk):
                ins = None
                for kc in range(8):
                    ins = e.matmul(bk[:, 0:NEXP], lhsT=h2T[:, kc, :], rhs=wr[:, kc, :], start=(kc == 0), stop=(kc == 7))
                return ins
            S.op("pe", lgm, r=[b_xres, b_wr], w=[bb])
            S.op("dve", lambda e, bk=bk: e.tensor_tensor(out=lg, in0=bk[:, 0:NEXP], in1=brt, op=ALU.add),
                 r=[bb, b_brt], w=[b_lg])
            dump("lg_%d" % gt, lg, [b_lg])
            S.op("dve", lambda e: e.max(out=mx8, in_=lg), r=[b_lg], w=[b_mx8])
            S.op("dve", lambda e: e.tensor_scalar(out=sm1[:, 0:1], in0=mx8[:, 0:1], scalar1=-1.0, scalar2=None,
                                                  op0=ALU.mult), r=[b_mx8], w=[b_sm1])
            S.op("dve", lambda e: e.tensor_scalar(out=msk, in0=lg, scalar1=mx8[:, 3:4], scalar2=None, op0=ALU.is_ge),
                 r=[b_lg, b_mx8], w=[b_msk])
            S.op("pool", lambda e: e.tensor_copy(out=mskb, in_=msk), r=[b_msk], w=[b_mskb])
            S.op("act", lambda e: e.activation(out=exq, in_=lg, func=AF.Exp, bias=sm1[:, 0:1]),
                 r=[b_lg, b_sm1], w=[b_exq])
            S.op("dve", lambda e: e.scalar_tensor_tensor(out=exq, in0=exq, scalar=1.0, in1=msk,
                                                         op0=ALU.mult, op1=ALU.mult, accum_out=sm1[:, 1:2]),
                 r=[b_exq, b_msk], w=[b_exq, b_sm1])
            S.op("dve", lambda e: e.reciprocal(out=sm1[:, 2:3], in_=sm1[:, 1:2]), r=[b_sm1], w=[b_sm1])
            S.op("dve", lambda e: e.tensor_scalar(out=P_all[:, gt, :], in0=exq, scalar1=sm1[:, 2:3], scalar2=None,
                                                  op0=ALU.mult), r=[b_exq, b_sm1], w=[b_Pall])
            bk2, bb2 = pjbank()

            def posm(e, bk2=bk2):
                e.matmul(bk2[:, 0:NEXP], lhsT=ustr, rhs=mskb, start=True, stop=True)
                return e.matmul(bk2[:, NEXP:2 * NEXP], lhsT=onesb, rhs=mskb, start=True, stop=True)
            S.op("pe", posm, r=[b_ustr, b_onesb, b_mskb], w=[bb2])
            S.op("dve", lambda e, bk2=bk2: e.tensor_tensor(out=slot, in0=bk2[:, 0:NEXP], in1=basec, op=ALU.add),
                 r=[bb2, b_basec], w=[b_slot])
            S.op("dve", lambda e: e.tensor_scalar(out=slot, in0=slot, scalar1=float(CAP - 1), scalar2=None, op0=ALU.min),
                 r=[b_slot], w=[b_slot])
            S.op("dve", lambda e: e.tensor_tensor(out=slot, in0=slot, in1=ecap, op=ALU.add),
                 r=[b_slot, b_ecap], w=[b_slot])
            S.op("dve", lambda e, bk2=bk2: e.tensor_tensor(out=basec, in0=bk2[:, NEXP:2 * NEXP], in1=basec, op=ALU.add),
                 r=[bb2, b_basec], w=[b_basec])
            for k in range(4):
                S.op("dve", lambda e, k=k: e.tensor_scalar(out=oh, in0=lg, scalar1=mx8[:, k:k + 1], scalar2=None,
                                                           op0=ALU.is_equal), r=[b_lg, b_mx8], w=[b_oh])
                S.op("dve", lambda e, k=k: e.scalar_tensor_tensor(out=j32, in0=oh, scalar=1.0, in1=slot,
                                                                  op0=ALU.mult, op1=ALU.mult, accum_out=destf[:, k:k + 1]),
                     r=[b_oh, b_slot], w=[b_j32, b_destf])
                S.op("dve", lambda e, k=k: e.scalar_tensor_tensor(out=j32, in0=oh, scalar=1.0, in1=P_all[:, gt, :],
                                                                  op0=ALU.mult, op1=ALU.mult,
                                                                  accum_out=pk_all[:, gt, k:k + 1]),
                     r=[b_oh, b_Pall], w=[b_j32, b_pkall])
            S.op("dve", lambda e: e.tensor_copy(out=desti[:, gt, :], in_=destf), r=[b_destf], w=[b_desti])
            for k in range(4):
                S.op("pool", lambda e, k=k: e.indirect_dma_start(
                    out=xg_d[:, :], out_offset=bass.IndirectOffsetOnAxis(ap=desti[:, gt, k:k + 1], axis=0),
                    in_=h2b[:, :], in_offset=None, bounds_check=bcreg(e), oob_is_err=False),
                    r=[b_h2b, b_desti], w=[b_xg], dma=True)

        MIX = {
            "A": dict(H=4, q=(qA, b_qA), pre=(preA, b_preA), lf=(lfA, b_lfA), Ep=(EpA, b_EpA), Em=(EmA, b_EmA),
                      qe=qeA, ke=keA, a=aA, v=(vA, b_vA), kd=(kdA, b_kdA), W=(WA, b_WA), V=(VA, b_VA),
                      ts=(tsA, b_tsA), Sf=(SfA, b_SfA), Sb=(SbA, b_SbA), sg=(sgA, b_sgA), rd=(rdA, b_rdA),
                      sgn=1.0, nw=V_HNW),
            "B": dict(H=2, q=(qB, b_qB), pre=(preB, b_preB), lf=(sB, b_sB), Ep=(EpB, b_EpB), Em=(EmB, b_EmB),
                      qe=qeB, ke=keB, a=aB, v=(vB, b_vB), kd=(kdB, b_kdB), W=(WB, b_WB), V=(VB, b_VB),
                      ts=(tsB, b_tsB), Sf=(SfB, b_SfB), Sb=(SbB, b_SbB), sg=(sgB, b_sgB), rd=(rdB, b_rdB),
                      sgn=-1.0 / 16.0, nw=V_GNW),
        }

        def load_norm_transpose(src_rows, j):
            for t in range(2):
                xa, xb_ = xt[t]
                na, nb_ = xnb[t]
                S.op("sp", lambda e, xa=xa, t=t: e.dma_start(out=xa, in_=src_rows[t * 128:(t + 1) * 128, :]),
                     w=[xb_], dma=True)
                S.op("act", lambda e, xa=xa, t=t: e.activation(out=junk, in_=xa, func=AF.Square,
                                                                accum_out=ss[:, 4 + t:5 + t]),
                     r=[xb_], w=[b_junk, b_ssr])
                S.op("act", lambda e, t=t: e.activation(out=ss[:, t:t + 1], in_=ss[:, 4 + t:5 + t], func=AF.Copy),
                     r=[b_ssr], w=[b_ss])
                S.op("pool", lambda e, t=t: e.tensor_scalar(out=ss[:, t:t + 1], in0=ss[:, t:t + 1], scalar1=1.0 / D,
                                                            scalar2=EPS, op0=ALU.mult, op1=ALU.add),
                     r=[b_ss], w=[b_ss])
                S.op("pool", lambda e, t=t: e.tensor_tensor(out=ss[:, t:t + 1], in0=ss[:, t:t + 1],
                                                            in1=neghalf[:, 0:1], op=ALU.pow),
                     r=[b_ss, b_neghalf], w=[b_ss])
                S.op("dve", lambda e, xa=xa, na=na, t=t: e.tensor_scalar(out=na, in0=xa, scalar1=ss[:, t:t + 1],
                                                                        scalar2=None, op0=ALU.mult),
                     r=[xb_, b_ss], w=[nb_])
                tpv = banks[0][:, :].bitcast(BF16).rearrange("p (k n) -> p k n", k=8)

                def tr(e, na=na):
                    ins = None
                    for kc in range(8):
                        ins = e.transpose(out=tpv[:, kc, :], in_=na[:, kc * 128:(kc + 1) * 128], identity=identb)
                    return ins
                S.op("pe", tr, r=[nb_, b_identb], w=[PB[0]])
                for kc in range(8):
                    S.op("act", lambda e, kc=kc, t=t: e.activation(
                        out=hT[:, kc, t * 128:(t + 1) * 128], in_=tpv[:, kc, :], func=AF.Identity,
                        scale=A1[:, j, kc:kc + 1], bias=modT[:, kc, j:j + 1]),
                        r=[PB[0], b_A1, b_modT], w=[b_hT])

        def proj_fm(c0, ncols, msize, consumer, mlist=None):
            wt, wb_ = wload(winb_d[:, c0:c0 + ncols].rearrange("(k p) n -> p k n", p=128),
                            view=lambda w: w[:, :, 0:ncols])
            nm = ncols // msize
            for m in (mlist if mlist is not None else range(nm)):
                bk, bb = pjbank()

                def mm(e, m=m, bk=bk):
                    ins = None
                    for kc in range(8):
                        ins = e.matmul(bk[0:msize, 0:ST], lhsT=wt[:, kc, m * msize:(m + 1) * msize],
                                       rhs=hT[:, kc, :], start=(kc == 0), stop=(kc == 7))
                    return ins
                S.op("pe", mm, r=[wb_, b_hT], w=[bb])
                consumer(m, bk, bb)

        def proj_tm(c0, dst, b_dst):
            wt, wb_ = wload(winb_d[:, c0:c0 + 512].rearrange("(k p) n -> p k n", p=128))
            for t in range(2):
                bk, bb = pjbank()

                def mm(e, t=t, bk=bk):
                    ins = None
                    for kc in range(8):
                        ins = e.matmul(bk[:, :], lhsT=hT[:, kc, t * 128:(t + 1) * 128], rhs=wt[:, kc, :],
                                       start=(kc == 0), stop=(kc == 7))
                    return ins
                S.op("pe", mm, r=[wb_, b_hT], w=[bb])
                S.op("act", lambda e, t=t, bk=bk: e.activation(out=dst[:, t, :], in_=bk[:, :], func=AF.Copy),
                     r=[bb], w=[b_dst])

        def act_evac(dst, b_dst, func, scale=1.0):
            def c(m, bk, bb):
                S.op("act", lambda e: e.activation(out=dst[:, m, :], in_=bk[:, 0:ST], func=func, scale=scale),
                     r=[bb], w=[b_dst])
            return c

        def gates_gen(X, d, need_q):
            M = MIX[X]
            H = M["H"]
            pre, b_pre = M["pre"]
            lf, b_lf = M["lf"]
            Ep, b_Ep = M["Ep"]
            Em, b_Em = M["Em"]
            a, b_a = M["a"][d]
            sgn = M["sgn"]
            pre2 = pre.rearrange("p h t -> p (h t)")
            lf2 = lf.rearrange("p h t -> p (h t)")
            m2 = m01.rearrange("p h t -> p (h t)")[:, 0:H * ST]
            S.op("dve", lambda e: e.tensor_tensor_scan(out=pre2, data0=m2, data1=lf2, initial=0.0,
                                                       op0=ALU.mult, op1=ALU.add),
                 r=[b_lf, b_m01], w=[b_pre])
            yield
            totv = pre.rearrange("p h (c t) -> p h c t", c=2)[:, :, :, 127]
            S.op("act", lambda e: e.activation(out=a, in_=totv, func=AF.Exp, scale=sgn), r=[b_pre], w=[b_a])
            if d == 0:
                sp_, sm_ = sgn, -sgn
            else:
                S.op("dve", lambda e: e.tensor_tensor(out=pre2, in0=pre2, in1=lf2, op=ALU.subtract),
                     r=[b_pre, b_lf], w=[b_pre])
                sp_, sm_ = -sgn, sgn
            S.op("act", lambda e: e.activation(out=Em, in_=pre, func=AF.Exp, scale=sm_), r=[b_pre], w=[b_Em])
            ke, b_ke = M["ke"][d]
            if X == "A":
                S.op("dve", lambda e: e.scalar_tensor_tensor(out=ke, in0=fA, scalar=1.0, in1=Em, op0=ALU.subtract,
                                                             op1=ALU.mult), r=[b_fA, b_Em], w=[b_ke])
            else:
                S.op("dve", lambda e: e.tensor_tensor(out=ke, in0=kB, in1=Em, op=ALU.mult),
                     r=[b_kB, b_Em], w=[b_ke])
            yield
            qmode = dbg.get("qmode", 2)
            if need_q and qmode >= 1 and X in dbg.get("qmix", ("A", "B")):
                S.op("act", lambda e: e.activation(out=Ep, in_=pre, func=AF.Exp, scale=sp_), r=[b_pre], w=[b_Ep])
                q, b_q = M["q"]
                qe, b_qe = M["qe"][d]
                if qmode >= 2 and X == "A":
                    S.op("dve", lambda e: e.tensor_tensor(out=qe, in0=q, in1=Ep, op=ALU.mult),
                         r=[b_q, b_Ep], w=[b_qe])
                elif qmode >= 2:
                    for par in range(2):
                        S.op("dve", lambda e, par=par: e.scalar_tensor_tensor(
                            out=qe[:, par], in0=Ep, scalar=pm[:, par:par + 1], in1=q, op0=ALU.mult, op1=ALU.mult),
                            r=[b_q, b_Ep, b_pm], w=[b_qe])

        def gates(X, d, need_q):
            for _ in gates_gen(X, d, need_q):
                pass

        def zproj_gen(d, need_q, last_hT_use=False):
            c0 = 512 + 512 * d
            proj_fm(c0, 512, 128, act_evac(fA, b_fA, AF.Sigmoid))
            if last_hT_use:
                run_prefetch()
            for h in range(4):
                S.op("dve", lambda e, h=h: e.tensor_scalar(out=fA[:, h, :], in0=fA[:, h, :],
                                                           scalar1=lbt[:, 8 + d * 4 + h:8 + d * 4 + h + 1],
                                                           scalar2=lbt[:, d * 4 + h:d * 4 + h + 1],
                                                           op0=ALU.mult, op1=ALU.add),
                     r=[b_fA, b_lbt], w=[b_fA])
            yield
            S.op("act", lambda e: e.activation(out=lfA, in_=fA, func=AF.Ln), r=[b_fA], w=[b_lfA])
            yield
            yield from gates_gen("A", d, need_q)

        def zproj(d, need_q, last_hT_use=False):
            for _ in zproj_gen(d, need_q, last_hT_use):
                pass

        def rproj():
            wt, wb_ = wload(winb_d[:, 3584:3616].rearrange("(k p) n -> p k n", p=128), view=lambda w: w[:, :, 0:32])
            bk, bb = pjbank()

            def mm(e):
                ins = None
                for d in range(2):
                    for kc in range(8):
                        ins = e.matmul(bk[0:16, d * ST:(d + 1) * ST], lhsT=wt[:, kc, d * 16:(d + 1) * 16],
                                       rhs=hT[:, kc, :], start=(kc == 0), stop=(kc == 7))
                return ins
            S.op("pe", mm, r=[wb_, b_hT], w=[bb])
            S.op("act", lambda e: e.activation(out=rT[0:16].rearrange("p d t -> p (d t)"), in_=bk[0:16, :],
                                               func=AF.Copy), r=[bb], w=[b_rT])

        def gproj_gen(d, need_q):
            bk, bb = pjbank()

            def mm(e):
                ins = None
                for kt in range(2):
                    ins = e.matmul(bk[:, kt * ST:(kt + 1) * ST], lhsT=w2s[0:16, d, kt * 128:(kt + 1) * 128],
                                   rhs=rT[0:16, d, :], start=True, stop=True)
                return ins
            S.op("pe", mm, r=[b_w2s, b_rT], w=[bb])
            for kt in range(2):
                S.op("act", lambda e, kt=kt: e.activation(out=sB[:, kt, :], in_=bk[:, kt * ST:(kt + 1) * ST],
                                                          func=AF.Exp, scale=-1.0,
                                                          bias=negb[:, d * 2 + kt:d * 2 + kt + 1]),
                     r=[bb, b_negb], w=[b_sB])
            S.op("act", lambda e: e.activation(out=sB, in_=sB, func=AF.Ln, bias=1.0), r=[b_sB], w=[b_sB])
            yield
            yield from gates_gen("B", d, need_q)

        def gproj(d, need_q):
            for _ in gproj_gen(d, need_q):
                pass

        def state_products(X, d, c, bankbuf):
            M = MIX[X]
            H = M["H"]
            ke, b_ke = M["ke"][d]
            kd, b_kd = M["kd"]
            v, b_v = M["v"]
            bk, bb = bankbuf
            tpv = bk[:, :].bitcast(BF16)

            def tr(e):
                ins = None
                for h in range(H):
                    ins = e.transpose(out=tpv[:, h * 128:(h + 1) * 128], in_=ke[:, h, c * 128:(c + 1) * 128],
                                      identity=identb)
                return ins
            S.op("pe", tr, r=[b_ke, b_identb], w=[bb])
            S.op("act", lambda e: e.activation(out=kd.rearrange("p h t -> p (h t)"), in_=tpv[:, 0:H * 128],
                                               func=AF.Copy), r=[bb], w=[b_kd])

            def mm(e):
                ins = None
                for h in range(4):
                    if X == "A":
                        ins = e.matmul(bk[:, h * 128:(h + 1) * 128], lhsT=kd[:, h, :],
                                       rhs=v[:, c, h * 128:(h + 1) * 128], start=True, stop=True)
                    else:
                        p0 = (h % 2) * 64
                        ins = e.matmul(bk[p0:p0 + 64, (h // 2) * 128:(h // 2 + 1) * 128],
                                       lhsT=kd[:, h // 2, p0:p0 + 64],
                                       rhs=v[:, c, h * 128:(h + 1) * 128], start=True, stop=True)
                return ins
            S.op("pe", mm, r=[b_kd, b_v], w=[bb])
            return bk[:, 0:H * 128].rearrange("p (h t) -> p h t", h=H), bb

        def fwd_update(X, d, c, store_idx):
            M = MIX[X]
            H = M["H"]
            W, b_W = M["W"]
            a, b_a = M["a"][d]
            Sf, b_Sf = M["Sf"]
            P, bb = state_products(X, d, c, (banks[6], PB[6]))
            S.op("dve", lambda e: e.tensor_tensor(out=W, in0=P, in1=W, op=ALU.add), r=[bb, b_W], w=[b_W])
            S.op("dve", lambda e: e.tensor_tensor(out=W, in0=W,
                                                  in1=a[:, :, c].unsqueeze(2).to_broadcast([128, H, 128]),
                                                  op=ALU.mult), r=[b_W, b_a], w=[b_W])
            if store_idx is not None:
                S.op("pool", lambda e: e.tensor_copy(out=Sf[:, store_idx], in_=W), r=[b_W], w=[b_Sf])

        def bwd_update(X, d, c, need_sb):
            M = MIX[X]
            H = M["H"]
            V, b_V = M["V"]
            ts, b_ts = M["ts"]
            a, b_a = M["a"][d]
            Sb, b_Sb = M["Sb"]
            S.op("dve", lambda e: e.tensor_tensor(out=ts, in0=V,
                                                  in1=a[:, :, c].unsqueeze(2).to_broadcast([128, H, 128]),
                                                  op=ALU.mult), r=[b_V, b_a], w=[b_ts])
            if need_sb:
                S.op("pool", lambda e: e.tensor_copy(out=Sb, in_=ts), r=[b_ts], w=[b_Sb])
            P, bb = state_products(X, d, c, (banks[6], PB[6]))
            S.op("dve", lambda e: e.tensor_tensor(out=V, in0=P, in1=ts, op=ALU.add), r=[bb, b_ts], w=[b_V])

        pf = {"fn": None}

        def run_prefetch():
            f_ = pf["fn"]
            pf["fn"] = None
            if f_ is not None:
                f_()

        def interleave(chains, fillers):
            fillers = list(fillers)
            for ch in chains:
                for _ in ch:
                    if fillers:
                        fillers.pop(0)()
            for f_ in fillers:
                f_()

        def st_common(src_rows, j, p2, need_b):
            if p2:
                proj_fm(0, 512, 128, act_evac(qA, b_qA, AF.Copy, scale=-1.0))
            proj_tm(1536, vA, b_vA)
            if p2:
                proj_fm(2560, 512, 128, lambda m, bk, bb: (act_evac(qB, b_qB, AF.Copy, scale=-0.125)(m, bk, bb) if m < 2
                                                          else act_evac(kB, b_kB, AF.Copy, scale=-1.0)(m - 2, bk, bb)))
            else:
                proj_fm(2560, 512, 128, lambda m, bk, bb: act_evac(kB, b_kB, AF.Copy, scale=-1.0)(m - 2, bk, bb),
                        mlist=[2, 3])
            proj_tm(3072, vB, b_vB)
            rproj()

        def pass1_st(seq, is_ctx, sti):
            rproj()
            fillers = [
                lambda: proj_fm(2560, 512, 128, lambda m, bk, bb: act_evac(kB, b_kB, AF.Copy, scale=-1.0)(m - 2, bk, bb),
                                mlist=[2, 3]),
                lambda: proj_tm(1536, vA, b_vA),
                lambda: proj_tm(3072, vB, b_vB),
            ]
            interleave([zproj_gen(0, False)], fillers)
            if not is_ctx:
                run_prefetch()

            def fw(X, c):
                g = (0 if is_ctx else 2 + sti * 2) + c
                store = g + 1 - 2 if (g + 1 >= 2 and g + 1 - 2 < 16) else None
                return lambda: fwd_update(X, 0, c, store)

            def bw(X, c):
                return lambda: bwd_update(X, 1, c, False)
            if is_ctx:
                interleave([zproj_gen(1, False)], [fw("A", 0), fw("A", 1)])
                run_prefetch()
                interleave([gproj_gen(0, False)], [bw("A", 1), bw("A", 0)])
                interleave([gproj_gen(1, False)], [fw("B", 0), fw("B", 1)])
                bw("B", 1)()
                bw("B", 0)()
            else:
                interleave([gproj_gen(0, False)], [fw("A", 0), fw("A", 1)])
                fw("B", 0)()
                fw("B", 1)()

        def pass2_st(seq, sti):
            stop = dbg.get("p2_stop", 99)
            rproj()

            def sm_filler(dst, b_dst, c0, half):
                return lambda: proj_fm(c0 + half * 512, 512, 128,
                                       lambda m, bk, bb: act_evac(dst, b_dst, AF.Sigmoid)(half * 4 + m, bk, bb))
            fillers = [
                lambda: proj_fm(0, 512, 128, act_evac(qA, b_qA, AF.Copy, scale=-1.0)),
                lambda: proj_fm(2560, 512, 128, lambda m, bk, bb: (
                    act_evac(qB, b_qB, AF.Copy, scale=-0.125)(m, bk, bb) if m < 2
                    else act_evac(kB, b_kB, AF.Copy, scale=-1.0)(m - 2, bk, bb))),
                lambda: proj_tm(1536, vA, b_vA),
                lambda: proj_tm(3072, vB, b_vB),
                lambda: proj_fm(2048, 512, 128, act_evac(sgA, b_sgA, AF.Silu)),
                lambda: proj_fm(3616, 512, 128, act_evac(sgB, b_sgB, AF.Silu)),
                sm_filler(smA, b_smA, 4128, 0), sm_filler(smA, b_smA, 4128, 1),
                sm_filler(smB, b_smB, 5152, 0), sm_filler(smB, b_smB, 5152, 1),
            ]
            fillers.append(run_prefetch)
            interleave([zproj_gen(0, True), zproj_gen(1, True), gproj_gen(0, True), gproj_gen(1, True)], fillers)
            run_prefetch()
            if stop <= 3:
                return
            for c in (1, 0):
                cl = sti * 2 + c
                cs = slice(c * 128, (c + 1) * 128)
                for X in dbg.get("p2_mixers", ("A", "B")):
                    M = MIX[X]
                    v, b_v = M["v"]
                    Sf, b_Sf = M["Sf"]
                    Sb, b_Sb = M["Sb"]
                    sg, b_sg = M["sg"]
                    rd, b_rd = M["rd"]
                    bwd_update(X, 1, c, True)
                    if stop <= 3.2:
                        continue
                    for d in range(2):
                        ke, b_ke = M["ke"][d]
                        qe, b_qe = M["qe"][d]

                        def scm(e, ke=ke, qe=qe, X=X, cs=cs):
                            ins = None
                            for h in range(4):
                                if X == "A":
                                    ins = e.matmul(banks[4][:, h * 128:(h + 1) * 128], lhsT=ke[:, h, cs], rhs=qe[:, h, cs],
                                                   start=True, stop=True)
                                else:
                                    ins = e.matmul(banks[4][:, h * 128:(h + 1) * 128], lhsT=ke[:, h // 2, cs],
                                                   rhs=qe[:, h % 2, h // 2, cs], start=True, stop=True)
                            return ins
                        S.op("pe", scm, r=[b_ke, b_qe], w=[PB[4]])
                        dst, b_dst, msk, b_msk = (scF, b_scF, maskF, b_maskF) if d == 0 else (scBt, b_scBt, maskB, b_maskB)
                        S.op("dve", lambda e, dst=dst, msk=msk: e.tensor_tensor(
                            out=dst, in0=banks[4][:, :].rearrange("p (h t) -> p h t", h=4), in1=msk, op=ALU.mult),
                            r=[PB[4], b_msk], w=[b_dst])
                    if stop <= 3.4:
                        continue
                    qf, b_qf = M["qe"][0]
                    qb_, b_qb = M["qe"][1]

                    def om(e, X=X, v=v, Sf=Sf, Sb=Sb, qf=qf, qb_=qb_, c=c, cs=cs, cl=cl):
                        ins = None
                        for h in range(4):
                            o = banks[5][:, h * 128:(h + 1) * 128]
                            vv = v[:, c, h * 128:(h + 1) * 128]
                            e.matmul(o, lhsT=vv, rhs=scF[:, h, :], start=True, stop=False)
                            e.matmul(o, lhsT=vv, rhs=scBt[:, h, :], start=False, stop=False)
                            if X == "A":
                                e.matmul(o, lhsT=Sf[:, cl, h, :], rhs=qf[:, h, cs], start=False, stop=False)
                                ins = e.matmul(o, lhsT=Sb[:, h, :], rhs=qb_[:, h, cs], start=False, stop=True)
                            else:
                                kt = h // 2
                                e.matmul(o, lhsT=Sf[:, cl, kt, :], rhs=qf[:, h % 2, kt, cs],
                                         start=False, stop=False)
                                ins = e.matmul(o, lhsT=Sb[:, kt, :], rhs=qb_[:, h % 2, kt, cs],
                                               start=False, stop=True)
                        return ins
                    S.op("pe", om, r=[b_v, b_scF, b_scBt, b_Sf, b_Sb, b_qf, b_qb], w=[PB[5]])
                    if stop <= 3.6:
                        continue
                    o5 = banks[5][:, :].rearrange("p (h t) -> p h t", h=4)
                    S.op("dve", lambda e: e.tensor_copy(out=oTs, in_=o5), r=[PB[5]], w=[b_oTs])
                    S.op("act", lambda e: e.activation(out=sq, in_=oTs, func=AF.Square), r=[b_oTs], w=[b_sq])
                    if X == "A" and c == 1:
                        dump("oT_%d_%d" % (seq, sti), oTs, [b_oTs])
                    S.op("pe", lambda e: e.matmul(banks[7][:, :], lhsT=onesb, rhs=sq.rearrange("p h t -> p (h t)"),
                                                  start=True, stop=True), r=[b_onesb, b_sq], w=[PB[7]])
                    if stop <= 3.7:
                        continue
                    r2 = rstd.rearrange("p h t -> p (h t)")
                    S.op("act", lambda e: e.activation(out=r2, in_=banks[7][:, :], func=AF.Ln, scale=1.0 / 128.0,
                                                       bias=EPS), r=[PB[7]], w=[b_rstd])
                    S.op("act", lambda e: e.activation(out=r2, in_=r2, func=AF.Exp, scale=-0.5), r=[b_rstd], w=[b_rstd])
                    if stop <= 3.8:
                        continue
                    S.op("dve", lambda e: e.tensor_tensor(out=oTs, in0=oTs, in1=rstd, op=ALU.mult),
                         r=[b_oTs, b_rstd], w=[b_oTs])
                    nwc = M["nw"]
                    S.op("dve", lambda e, rd=rd, sg=sg, nwc=nwc, cs=cs: e.scalar_tensor_tensor(
                        out=rd[:, :, cs], in0=oTs, scalar=vecs[:, nwc:nwc + 1], in1=sg[:, :, cs],
                        op0=ALU.mult, op1=ALU.mult), r=[b_oTs, b_vecs, b_sg], w=[b_rd])
            dump("rdA_%d_%d" % (seq, sti), rdA, [b_rdA])
            dump("rdB_%d_%d" % (seq, sti), rdB, [b_rdB])
            if stop <= 4:
                return
            v4 = lambda w: w.rearrange("p k n -> p (k n)").rearrange("p (h n) -> p h n", h=4)
            wua, b_wua = wload(wupob_d[0:512, :].rearrange("(h p) n -> p h n", p=128), view=v4)
            wub, b_wub = wload(wupob_d[512:1024, :].rearrange("(h p) n -> p h n", p=128), view=v4)
            wua = v4(wua)
            wub = v4(wub)
            for m in range(8):
                bk, bb = pjbank()

                def um(e, m=m, bk=bk):
                    ins = None
                    for h in range(4):
                        ins = e.matmul(bk[:, 0:ST], lhsT=wua[:, h, m * 128:(m + 1) * 128], rhs=rdA[:, h, :],
                                       start=(h == 0), stop=(h == 3))
                    for h in range(4):
                        ins = e.matmul(bk[:, ST:2 * ST], lhsT=wub[:, h, m * 128:(m + 1) * 128], rhs=rdB[:, h, :],
                                       start=(h == 0), stop=(h == 3))
                    return ins
                S.op("pe", um, r=[b_wua, b_wub, b_rdA, b_rdB], w=[bb])
                S.op("dve", lambda e, m=m, bk=bk: e.tensor_tensor(out=mt1, in0=bk[:, 0:ST], in1=smA[:, m, :], op=ALU.mult),
                     r=[bb, b_smA], w=[b_mt1])
                S.op("dve", lambda e, m=m, bk=bk: e.tensor_tensor(out=mt2, in0=bk[:, ST:2 * ST], in1=smB[:, m, :],
                                                                 op=ALU.mult), r=[bb, b_smB], w=[b_mt2])
                S.op("pool", lambda e, m=m: e.tensor_tensor(out=mrgT[:, m, :], in0=mt1, in1=mt2, op=ALU.add),
                     r=[b_mt1, b_mt2], w=[b_mrgT])
            dump("mrgT_%d_%d" % (seq, sti), mrgT, [b_mrgT])
            if stop <= 5:
                return
            wo = [wload(wupob_d[1024:2048, n * 512:(n + 1) * 512].rearrange("(k p) n -> p k n", p=128)) for n in range(2)]
            for t in range(2):
                for n in range(2):
                    bk, bb = pjbank()
                    won, b_won = wo[n]

                    def omm(e, t=t, bk=bk, won=won):
                        ins = None
                        for kc in range(8):
                            ins = e.matmul(bk[:, :], lhsT=mrgT[:, kc, t * 128:(t + 1) * 128], rhs=won[:, kc, :],
                                           start=(kc == 0), stop=(kc == 7))
                        return ins
                    S.op("pe", omm, r=[b_mrgT, b_won], w=[bb])
                    S.op("act", lambda e, n=n, bk=bk: e.activation(out=mixs[:, n * 512:(n + 1) * 512], in_=bk[:, :],
                                                                    func=AF.Copy), r=[bb], w=[b_mixs])
                r0 = seq * LAT + sti * ST + t * 128
                S.op("sp", lambda e, r0=r0: e.dma_start(out=xres, in_=x_d[r0:r0 + 128, :]), w=[b_xres], dma=True)
                S.op("act", lambda e: e.activation(out=junk, in_=mixs, func=AF.Square, accum_out=st2[:, 4:5]),
                     r=[b_mixs], w=[b_junk, b_st2r])
                S.op("act", lambda e: e.activation(out=st2[:, 0:1], in_=st2[:, 4:5], func=AF.Copy),
                     r=[b_st2r], w=[b_st2])
                S.op("pool", lambda e: e.tensor_scalar(out=st2[:, 0:1], in0=st2[:, 0:1], scalar1=1.0 / D, scalar2=EPS,
                                                       op0=ALU.mult, op1=ALU.add), r=[b_st2], w=[b_st2])
                S.op("pool", lambda e: e.tensor_tensor(out=st2[:, 0:1], in0=st2[:, 0:1], in1=neghalf[:, 0:1], op=ALU.pow),
                     r=[b_st2, b_neghalf], w=[b_st2])
                S.op("dve", lambda e: e.tensor_tensor(out=mixs, in0=mixs, in1=G1, op=ALU.mult),
                     r=[b_mixs, b_G1], w=[b_mixs])
                S.op("dve", lambda e: e.scalar_tensor_tensor(out=x1t, in0=mixs, scalar=st2[:, 0:1], in1=xres,
                                                             op0=ALU.mult, op1=ALU.add),
                     r=[b_mixs, b_st2, b_xres], w=[b_x1t])
                S.op("sp", lambda e, r0=r0: e.dma_start(out=x1_d[r0:r0 + 128, :], in_=x1t), r=[b_x1t], w=[b_x1d],
                     dma=True)
                if dbg.get("moe", True):
                    m1_tile(r0 // 128)

        b_x1d = Buf("x1d")
        nseq = dbg.get("nseq", NSEQ)
        p2list = dbg.get("p2list", list(range(7, -1, -1)))
        sts = []
        for seq in range(nseq):
            sts.append(("p1", seq, True, 0))
            for sti in range(dbg.get("p1n", 8)):
                sts.append(("p1", seq, False, sti))
            for sti in p2list:
                sts.append(("p2", seq, False, sti))

        def emit_load(desc):
            kind, seq, is_ctx, sti = desc
            if is_ctx:
                load_norm_transpose(ctx_d[seq * CTXL:(seq + 1) * CTXL, :], 2)
            else:
                load_norm_transpose(x_d[seq * LAT + sti * ST:seq * LAT + (sti + 1) * ST, :], seq)

        emit_load(sts[0])
        for i, desc in enumerate(sts):
            kind, seq, is_ctx, sti = desc
            if is_ctx:
                for X in ("A", "B"):
                    for key in ("W", "V"):
                        ap_, bf_ = MIX[X][key]
                        S.op("pool", lambda e, ap_=ap_: e.memset(ap_, 0.0), w=[bf_])
                S.op("sp", lambda e, seq=seq: e.dma_start(out=G1, in_=bc_d[seq, 0:1, :].to_broadcast([128, D])),
                     r=[b_bcd], w=[b_G1], dma=True)
                S.op("sp", lambda e, seq=seq: e.dma_start(out=SH2, in_=bc_d[seq, 1:2, :].to_broadcast([128, D])),
                     r=[b_bcd], w=[b_SH2], dma=True)
                S.op("sp", lambda e, seq=seq: e.dma_start(out=A2, in_=bc_d[seq, 2:3, :].to_broadcast([128, D])),
                     r=[b_bcd], w=[b_A2], dma=True)
            if i + 1 < len(sts):
                pf["fn"] = (lambda d_=sts[i + 1]: emit_load(d_))
            if kind == "p1":
                pass1_st(seq, is_ctx, sti)
                if is_ctx and seq == 0:
                    dump("WA_ctx", WA, [b_WA])
                    dump("VA_ctx", VA, [b_VA])
                    dump("WB_ctx", WB, [b_WB])
                    dump("VB_ctx", VB, [b_VB])
            else:
                pass2_st(seq, sti)
            run_prefetch()
        if "x1" in dbg_out:
            xr0, xr1 = dbg.get("x1rows", (0, NSEQ * LAT))
            S.op("sp", lambda e: e.dma_start(out=dbg_out["x1"], in_=x1_d[xr0:xr1, :]), r=[b_x1d], w=[Buf("o")], dma=True)

        if dbg.get("moe", True) and dbg.get("m2", True):
            A.release(mix_mark)
            S.fence()
            NWB = dbg.get("nwb", 3)
            wts = [[T([8, 1024], BF16, "w%s%d" % (nm, i)) for nm in ("g", "u", "d")] for i in range(NWB)]
            xgt = [T([2, 1024], BF16, "xgt%d" % i) for i in range(2)]
            xgTs = [T([8, GR], BF16, "xgT%d" % i) for i in range(2)]
            xgT1bufs = [Buf("xgT1_%d" % i) for i in range(2)]
            actTs = [T([8, GR], BF16, "actT%d" % i) for i in range(2)]
            rrb = [T([GR], F32, "rr%d" % i) for i in range(2)]
            sgb = [T([GR], F32, "sg%d" % i) for i in range(2)]
            u1b = [T([GR], F32, "u1%d" % i) for i in range(2)]
            t1b = [T([GR], F32, "t1%d" % i) for i in range(2)]
            ytl = [T([1024], F32, "yt%d" % i) for i in range(2)]
            bg7, b_bg7 = T([256], F32, "bg7")
            S.op("dve", lambda e: e.tensor_scalar(out=bg7, in0=vecs[:, V_BG:V_BG + 256], scalar1=-1.0, scalar2=7.0,
                                                  op0=ALU.mult, op1=ALU.add), r=[b_vecs], w=[b_bg7])
            elist = dbg.get("elist", list(range(NEXP)))
            ngr = dbg.get("ngr", NGR)
            use_guard = dbg.get("guard", True)
            S.gran = GR
            S.op("dve", lambda e: e.tensor_copy(out=cnti, in_=basec), r=[b_basec], w=[b_cnti])
            gctr = [0]
            guc = [0]
            dnc = [0]
            for ei, ex in enumerate(elist):
                (wg, b_wg), (wu, b_wu), (wd, b_wd) = wts[ei % NWB]
                for (wt_, wb__, src) in ((wg, b_wg, wg_d), (wu, b_wu, wu_d), (wd, b_wd, wd_d)):
                    S.op("pool", lambda e, wt_=wt_, src=src, ex=ex: e.dma_start(
                        out=wt_, in_=src[ex].rearrange("(k p) n -> p k n", p=128)), w=[wb__], dma=True)
                if use_guard:
                    for en in ("pe", "act", "dve", "sp"):
                        def ldc(e, ex=ex, en=en):
                            if en not in S.cnt_regs:
                                S.cnt_regs[en] = e.alloc_register("cnt_" + en)
                            e.load(S.cnt_regs[en], cnti[0:1, ex:ex + 1])
                            return e.drain()
                        S.op(en, ldc, r=[b_cnti])
                def make_granule(ex, g, wg, wu, wd, b_wg, b_wu, b_wd):
                    r0 = ex * CAP + g * GR
                    par = gctr[0] % 2
                    gctr[0] += 1
                    xa, xb_ = xgt[par]
                    xgT, b_xgT = xgTs[par]
                    b_xgT1 = xgT1bufs[par]
                    actT, b_actT = actTs[par]

                    def part_load_tr(GD):
                        S.op("sp", lambda e, xa=xa, r0=r0: e.dma_start(
                            out=xa, in_=xg_d[r0:r0 + GR, :].rearrange("(t p) n -> p t n", p=128)),
                            r=[b_xg], w=[xb_], dma=True, guard=GD)
                        for hf in range(2):
                            tb = 4 + hf
                            tpv = banks[tb][:, :].bitcast(BF16).rearrange("p (k n) -> p k n", k=4)

                            def tr(e, xa=xa, hf=hf, tpv=tpv):
                                ins = None
                                for q in range(4):
                                    kc = hf * 4 + q
                                    for t in range(2):
                                        ins = e.transpose(out=tpv[:, q, t * 128:(t + 1) * 128],
                                                          in_=xa[:, t, kc * 128:(kc + 1) * 128], identity=identb)
                                return ins
                            S.op("pe", tr, r=[xb_, b_identb], w=[PB[tb]], guard=GD)
                            if hf == 0:
                                S.op("act", lambda e, hf=hf, tpv=tpv, xgT=xgT: e.activation(
                                    out=xgT[:, hf * 4:(hf + 1) * 4, :], in_=tpv, func=AF.Copy), r=[PB[tb]], w=[b_xgT], guard=GD)
                            else:
                                S.op("dve", lambda e, hf=hf, tpv=tpv, xgT=xgT: e.tensor_copy(
                                    out=xgT[:, hf * 4:(hf + 1) * 4, :], in_=tpv), r=[PB[tb]], w=[b_xgT1], guard=GD)

                    def part_fc(GD):
                        for fc in range(8):
                            bi = guc[0] % 2
                            guc[0] += 1
                            bk, bb = banks[bi], PB[bi]
                            bku, bbu = banks[2 + bi], PB[2 + bi]

                            def gmm(e, fc=fc, bk=bk, wg=wg, xgT=xgT):
                                ins = None
                                for kc in range(8):
                                    ins = e.matmul(bk[:, 0:GR], lhsT=wg[:, kc, fc * 128:(fc + 1) * 128], rhs=xgT[:, kc, :],
                                                   start=(kc == 0), stop=(kc == 7))
                                return ins

                            def umm(e, fc=fc, bku=bku, wu=wu, xgT=xgT):
                                ins = None
                                for kc in range(8):
                                    ins = e.matmul(bku[:, 0:GR], lhsT=wu[:, kc, fc * 128:(fc + 1) * 128], rhs=xgT[:, kc, :],
                                                   start=(kc == 0), stop=(kc == 7))
                                return ins
                            S.op("pe", gmm, r=[b_wg, b_xgT, b_xgT1], w=[bb], guard=GD)
                            S.op("pe", umm, r=[b_wu, b_xgT, b_xgT1], w=[bbu], guard=GD)
                            rr, b_rr = rrb[fc % 2]
                            sg_, b_sg_ = sgb[fc % 2]
                            u1, b_u1 = u1b[fc % 2]
                            t1, b_t1 = t1b[fc % 2]
                            cb = ex * 8 + fc
                            cu = V_BU + ex * 8 + fc
                            S.op("act", lambda e, bk=bk, rr=rr, cb=cb: e.activation(
                                out=rr, in_=bk[:, 0:GR], func=AF.Relu, scale=-1.0, bias=bg7[:, cb:cb + 1]),
                                r=[bb, b_bg7], w=[b_rr], guard=GD)
                            S.op("dve", lambda e, bku=bku, u1=u1, cu=cu: e.tensor_scalar(
                                out=u1, in0=bku[:, 0:GR], scalar1=vecs[:, cu:cu + 1], scalar2=7.0, op0=ALU.add, op1=ALU.min),
                                r=[bbu, b_vecs], w=[b_u1], guard=GD)
                            S.op("act", lambda e, rr=rr, sg_=sg_: e.activation(out=sg_, in_=rr, func=AF.Sigmoid, scale=-1.702,
                                                                               bias=1.702 * 7.0), r=[b_rr], w=[b_sg_], guard=GD)
                            S.op("dve", lambda e, u1=u1: e.tensor_scalar(out=u1, in0=u1, scalar1=-7.0, scalar2=1.0,
                                                                        op0=ALU.max, op1=ALU.add), r=[b_u1], w=[b_u1], guard=GD)
                            S.op("dve", lambda e, rr=rr, sg_=sg_, t1=t1: e.scalar_tensor_tensor(
                                out=t1, in0=rr, scalar=7.0, in1=sg_, op0=ALU.subtract, op1=ALU.mult),
                                r=[b_rr, b_sg_], w=[b_t1], guard=GD)
                            S.op("dve", lambda e, t1=t1, u1=u1, fc=fc, actT=actT: e.tensor_tensor(
                                out=actT[:, fc, :], in0=t1, in1=u1, op=ALU.mult), r=[b_t1, b_u1], w=[b_actT], guard=GD)

                    def part_dn(GD):
                        for t in range(2):
                            ya, yb_ = ytl[t]
                            for n in range(2):
                                bi = 6 + dnc[0] % 2
                                dnc[0] += 1
                                bk, bb = banks[bi], PB[bi]

                                def dn(e, t=t, n=n, bk=bk, wd=wd, actT=actT):
                                    ins = None
                                    for fc in range(8):
                                        ins = e.matmul(bk[:, :], lhsT=actT[:, fc, t * 128:(t + 1) * 128],
                                                       rhs=wd[:, fc, n * 512:(n + 1) * 512], start=(fc == 0), stop=(fc == 7))
                                    return ins
                                S.op("pe", dn, r=[b_actT, b_wd], w=[bb], guard=GD)
                                S.op("act", lambda e, ya=ya, n=n, bk=bk: e.activation(out=ya[:, n * 512:(n + 1) * 512],
                                                                                     in_=bk[:, :], func=AF.Copy, scale=-1.0),
                                     r=[bb], w=[yb_], guard=GD)
                            S.op("sp", lambda e, ya=ya, r0=r0, t=t: e.dma_start(out=yg_d[r0 + t * 128:r0 + (t + 1) * 128, :],
                                                                                in_=ya), r=[yb_], w=[b_yg], dma=True, guard=GD)

                    return part_load_tr, part_fc, part_dn

                grs = [make_granule(ex, g, wg, wu, wd, b_wg, b_wu, b_wd) for g in range(ngr)]
                gdf = (lambda g_, ex=ex: (ex, g_)) if use_guard else (lambda g_: None)
                grs[0][0](gdf(0))
                for g in range(ngr):
                    grs[g][1](gdf(g))
                    if g + 1 < ngr:
                        grs[g + 1][0](gdf(g))
                    grs[g][2](gdf(g))
            A.release(mix_mark)
            S.fence()
            yks = [[T([1024], F32, "yk%d_%d" % (k, i)) for k in range(4)] for i in range(2)]
            accs = [T([1024], F32, "acc%d" % i) for i in range(2)]
            x1rs = [T([1024], F32, "x1r%d" % i) for i in range(2)]
            PTs = [T([128], F32, "PT%d" % i) for i in range(2)]
            sqj, b_sqj = T([1024], BF16, "sqj")
            G2, b_G2 = T([1024], F32, "G2")
            bdn, b_bdn = T([1024], F32, "bdn")
            st3, b_st3 = T([8], F32, "st3")
            b_st3r = Buf("st3r")
            S.op("sp", lambda e: e.dma_start(out=bdn[0:NEXP], in_=bd_d), w=[b_bdn], dma=True)
            tlist = dbg.get("m3tiles", list(range(NTT)))
            curseq = [-1]
            def m3_bufs(ti):
                return yks[ti % 2], accs[ti % 2], x1rs[ti % 2], PTs[ti % 2]

            def m3_gather(ti, gt):
                yk, (acc, b_acc), (x1r, b_x1r), (PT, b_PT) = m3_bufs(ti)
                seq = gt // (LAT // 128)
                for k in range(4):
                    S.op("pool", lambda e, k=k, gt=gt, yk=yk: e.indirect_dma_start(
                        out=yk[k][0][:, :], out_offset=None, in_=yg_d[:, :],
                        in_offset=bass.IndirectOffsetOnAxis(ap=desti[:, gt, k:k + 1], axis=0),
                        bounds_check=bcreg(e), oob_is_err=False),
                        r=[b_yg, b_desti], w=[yk[k][1]], dma=True)
                S.op("sp", lambda e, gt=gt, x1r=x1r: e.dma_start(out=x1r, in_=x1_d[gt * 128:(gt + 1) * 128, :]),
                     r=[b_x1d], w=[b_x1r], dma=True)

            def m3_compute(ti, gt):
                yk, (acc, b_acc), (x1r, b_x1r), (PT, b_PT) = m3_bufs(ti)
                seq = gt // (LAT // 128)
                if seq != curseq[0]:
                    curseq[0] = seq
                    S.op("sp", lambda e, seq=seq: e.dma_start(out=G2, in_=bc_d[seq, 3:4, :].to_broadcast([128, D])),
                         r=[b_bcd], w=[b_G2], dma=True)
                bk, bb = pjbank()
                S.op("pe", lambda e, bk=bk, gt=gt: e.transpose(out=bk[0:NEXP, 0:128], in_=P_all[:, gt, :], identity=identf),
                     r=[b_Pall, b_identf], w=[bb])
                S.op("act", lambda e, bk=bk, PT=PT: e.activation(out=PT[0:NEXP], in_=bk[0:NEXP, 0:128], func=AF.Copy),
                     r=[bb], w=[b_PT])
                bks = []
                for n in range(2):
                    bk2, bb2 = pjbank()
                    S.op("pe", lambda e, bk2=bk2, n=n, PT=PT: e.matmul(bk2[:, :], lhsT=PT[0:NEXP], rhs=bdn[0:NEXP, n * 512:(n + 1) * 512],
                                                               start=True, stop=True), r=[b_PT, b_bdn], w=[bb2])
                    bks.append((bk2, bb2))
                S.op("dve", lambda e, gt=gt, acc=acc, yk=yk: e.tensor_scalar(out=acc, in0=yk[0][0], scalar1=pk_all[:, gt, 0:1], scalar2=None,
                                                             op0=ALU.mult), r=[yk[0][1], b_pkall], w=[b_acc])
                for k in range(1, 4):
                    S.op("dve", lambda e, k=k, gt=gt, acc=acc, yk=yk: e.scalar_tensor_tensor(out=acc, in0=yk[k][0], scalar=pk_all[:, gt, k:k + 1],
                                                                             in1=acc, op0=ALU.mult, op1=ALU.add),
                         r=[yk[k][1], b_pkall, b_acc], w=[b_acc])
                for n in range(2):
                    bk2, bb2 = bks[n]
                    S.op("dve", lambda e, bk2=bk2, n=n, acc=acc: e.tensor_tensor(out=acc[:, n * 512:(n + 1) * 512], in0=bk2[:, :],
                                                                        in1=acc[:, n * 512:(n + 1) * 512], op=ALU.add),
                         r=[bb2, b_acc], w=[b_acc])
                dump("ffn_%d" % gt, acc, [b_acc])

                S.op("act", lambda e, acc=acc: e.activation(out=sqj, in_=acc, func=AF.Square, accum_out=st3[:, 4:5]),
                     r=[b_acc], w=[b_sqj, b_st3r])
                S.op("act", lambda e: e.activation(out=st3[:, 0:1], in_=st3[:, 4:5], func=AF.Copy),
                     r=[b_st3r], w=[b_st3])
                S.op("pool", lambda e: e.tensor_scalar(out=st3[:, 0:1], in0=st3[:, 0:1], scalar1=1.0 / D, scalar2=EPS,
                                                       op0=ALU.mult, op1=ALU.add), r=[b_st3], w=[b_st3])
                S.op("pool", lambda e: e.tensor_tensor(out=st3[:, 0:1], in0=st3[:, 0:1], in1=neghalf[:, 0:1], op=ALU.pow),
                     r=[b_st3, b_neghalf], w=[b_st3])
                S.op("dve", lambda e, acc=acc: e.tensor_tensor(out=acc, in0=acc, in1=G2, op=ALU.mult), r=[b_acc, b_G2], w=[b_acc])
                S.op("dve", lambda e, acc=acc, x1r=x1r: e.scalar_tensor_tensor(out=acc, in0=acc, scalar=st3[:, 0:1], in1=x1r,
                                                             op0=ALU.mult, op1=ALU.add),
                     r=[b_acc, b_st3, b_x1r], w=[b_acc])
                S.op("sp", lambda e, gt=gt, acc=acc: e.dma_start(out=out_d[gt * 128:(gt + 1) * 128, :], in_=acc),
                     r=[b_acc], w=[Buf("outd")], dma=True)


            if tlist:
                m3_gather(0, tlist[0])
            for ti, gt in enumerate(tlist):
                if ti + 1 < len(tlist):
                    m3_gather(ti + 1, tlist[ti + 1])
                m3_compute(ti, gt)

        dump("P_all", P_all, [b_Pall])
        dump("pk_all", pk_all, [b_pkall])
        dump("desti", desti, [b_desti])
        print("arena high water", A.hi)

        fin = Buf("fin")
        last = []
        for e in ENGS:
            if S.ops[e]:
                last.append(S.ops[e][-1])
        alld = [o for e in ENGS for o in S.ops[e] if o.dma]
        fo = S.op("sp", lambda e: e.nop(), w=[fin])
        fo.deps.update(alld)
        fo.deps.update(last)
        fo.deps.discard(fo)

        S.finalize(nc, ctx)
        with nc.Block() as block:
            @block.tensor
            def _(e):
                S.emit("pe", e)

            @block.scalar
            def _(e):
                S.emit("act", e)

            @block.vector
            def _(e):
                S.emit("dve", e)

            @block.gpsimd
            def _(e):
                S.emit("pool", e)

            @block.sync
            def _(e):
                S.emit("sp", e)
    return nc


def _fm(v, n):
    return np.ascontiguousarray(np.asarray(v, np.float32).reshape(n, 128).T)


def make_core_inputs(core, inp):
    b0 = core * NSEQ
    x = np.ascontiguousarray(inp["x"][b0:b0 + NSEQ].reshape(NSEQ * LAT, D))
    cx = np.ascontiguousarray(inp["ctx"][b0:b0 + NSEQ].reshape(NSEQ * CTXL, D))
    cs = np.stack([inp["c"][b0], inp["c"][b0 + 1], inp["c_ctx"]], axis=-1)
    cT = np.ascontiguousarray(cs.reshape(8, 128, 3).transpose(1, 0, 2))
    vecs = np.zeros((128, NV), np.float32)
    vecs[:, V_BADA:V_BADA + 48] = _fm(inp["b_ada"][0], 48)
    vecs[:, V_GPRE:V_GPRE + 8] = _fm(inp["g_pre_mix"][0], 8)
    lb = inp["hgrn_lb"]
    for s in range(2):
        for d in range(2):
            vecs[:, V_LBR + s * 8 + d * 4:V_LBR + s * 8 + d * 4 + 4] = _fm(lb[s, d], 4)
    vecs[:, V_HNW] = inp["hgrn_norm_w"][0]
    vecs[:, V_GNW] = inp["gla_norm_w"][0]
    for d in range(2):
        vecs[:, V_GKB + d * 2:V_GKB + d * 2 + 2] = _fm(inp["gla_gk_b"][0, d], 2)
    vecs[:, V_BG:V_BG + 256] = np.asarray(inp["b_gate"][0]).reshape(32, 8, 128).transpose(2, 0, 1).reshape(128, 256)
    vecs[:, V_BU:V_BU + 256] = np.asarray(inp["b_up"][0]).reshape(32, 8, 128).transpose(2, 0, 1).reshape(128, 256)
    rowv = np.zeros((8, D), np.float32)
    ba = inp["b_ada"][0]
    rowv[0] = ba[2048:3072]
    rowv[1] = ba[3072:4096]
    rowv[2] = ba[4096:5120]
    rowv[3] = ba[5120:6144]
    rowv[4] = inp["g_post_mix"][0]
    rowv[5] = inp["g_pre_ffn"][0]
    rowv[6] = inp["g_post_ffn"][0]
    rowv[7, :32] = inp["b_router"][0]
    wupo = np.concatenate([inp["w_up_a"][0], inp["w_up_b"][0], inp["w_o"][0]], axis=0)
    return {
        "x": x, "ctx": cx, "cT": cT, "vecs": vecs, "rowv": rowv,
        "w_ada": np.ascontiguousarray(inp["w_ada"][0]), "w_in": np.ascontiguousarray(inp["w_in"][0]),
        "gk_w2": np.ascontiguousarray(inp["gla_gk_w2"][0]), "wupo": np.ascontiguousarray(wupo),
        "w_router": np.ascontiguousarray(inp["w_router"][0]),
        "w_gate": np.ascontiguousarray(inp["w_gate"][0]), "w_up": np.ascontiguousarray(inp["w_up"][0]),
        "w_down": np.ascontiguousarray(inp["w_down"][0]), "b_down": np.ascontiguousarray(inp["b_down"][0]),
    }


def kernel(**inputs):
    inp = {k: np.asarray(v) for k, v in inputs.items()}
    nc = build_program()
    in_maps = [make_core_inputs(c, inp) for c in range(8)]
    res = run_bass_kernel_spmd(nc, in_maps, core_ids=list(range(8)))
    outs = [np.asarray(r["out"]).reshape(NSEQ, LAT, D) for r in res.results]
    return np.concatenate(outs, axis=0).astype(np.float32)
```

```python
import numpy as np
from contextlib import ExitStack
import concourse.bass as bass
import concourse.mybir as mybir
from concourse.bass_utils import run_bass_kernel_spmd

F32 = mybir.dt.float32
BF16 = mybir.dt.bfloat16
U8 = mybir.dt.uint8
I32 = mybir.dt.int32
U32 = mybir.dt.uint32
AF = mybir.ActivationFunctionType
ALU = mybir.AluOpType

D = 1024
NSEQ = 2
LAT = 2048
CTXL = 256
ST = 256
INW = 6176
EPS = 1e-6
NEXP = 32

ENGS = ("pe", "act", "dve", "pool", "sp")
EPOCH = 16384
ND = 8


class Buf:
    __slots__ = ("name", "last_w", "readers")

    def __init__(self, name):
        self.name = name
        self.last_w = None
        self.readers = []


class Op:
    __slots__ = ("eng", "fn", "deps", "dma", "done", "gid", "inc", "guard")

    def __init__(self, eng, fn, dma):
        self.eng = eng
        self.fn = fn
        self.dma = dma
        self.deps = set()
        self.done = None
        self.inc = 0
        self.guard = None


class Sched:
    def __init__(self):
        self.ops = {e: [] for e in ENGS}
        self.n = 0
        self.fence_ops = set()
        self.cnt_regs = {}
        self.ext_cache = {}
        self.thr_regs = {}
        self.gran = 256

    def fence(self):
        f = set()
        for e in ENGS:
            comp = [o for o in self.ops[e] if not o.dma]
            if comp:
                f.add(comp[-1])
            dm = [o for o in self.ops[e] if o.dma]
            f.update(dm[-ND:])
        self.fence_ops = f

    def op(self, eng, fn, r=(), w=(), dma=False, guard=None):
        o = Op(eng, fn, dma)
        o.guard = guard
        o.gid = self.n
        self.n += 1
        for b in r:
            if b.last_w is not None:
                o.deps.add(b.last_w)
        for b in w:
            if b.last_w is not None:
                o.deps.add(b.last_w)
            for rd in b.readers:
                o.deps.add(rd)
        for b in r:
            b.readers.append(o)
        for b in w:
            b.last_w = o
            b.readers = []
        o.deps.update(self.fence_ops)
        o.deps.discard(o)
        self.ops[eng].append(o)
        return o

    def finalize(self, nc, ctx):
        self.sems = {}
        self.groups = {}
        for e in ENGS:
            for o in self.ops[e]:
                if o.guard is not None:
                    self.groups.setdefault(o.guard, []).append(o)
        for e in ENGS:
            kc = 0
            kd = 0
            dq = []
            for o in self.ops[e]:
                if o.dma:
                    slot = kd % ND
                    key = (e, "d", slot)
                    if key not in self.sems:
                        self.sems[key] = ctx.enter_context(nc.semaphore("sd_%s_%d" % (e, slot)))
                    o.done = (self.sems[key], 16 * (kd // ND + 1))
                    if kd >= ND:
                        o.deps.add(dq[kd - ND])
                    dq.append(o)
                    kd += 1
                else:
                    ep = kc // EPOCH
                    key = (e, "c", ep)
                    if key not in self.sems:
                        self.sems[key] = ctx.enter_context(nc.semaphore("sc_%s_%d" % (e, ep)))
                    o.done = (self.sems[key], kc % EPOCH + 1)
                    kc += 1

    def emit(self, eng_name, eng):
        known = {}

        def emit_one(o, known):
            for d in sorted(o.deps, key=lambda x: -x.gid):
                sm, v = d.done
                k = id(sm)
                if known.get(k, 0) >= v:
                    continue
                eng.wait_ge(sm, v)
                known[k] = v
            ins = o.fn(eng)
            sm, v = o.done
            ins.then_inc(sm, 16 if o.dma else 1)

        def thr_reg(g):
            key = (eng_name, g)
            if key not in self.thr_regs:
                rg = eng.alloc_register("thr_%s_%d" % (eng_name, g))
                eng.reg_mov(rg, g * self.gran)
                self.thr_regs[key] = rg
            return self.thr_regs[key]

        def emit_chain(chain, known):
            gd, block = chain[0]
            rest = chain[1:]
            thr = thr_reg(gd[1])
            cnt = self.cnt_regs[eng_name]
            with eng.If_lt(thr, cnt):
                kn = dict(known)
                for b in block:
                    emit_one(b, kn)
                if rest:
                    emit_chain(rest, kn)
            with eng.Else():
                gset = frozenset(g_ for g_, _ in chain)
                ck = (gset, )
                if ck not in self.ext_cache:
                    ext = {}
                    for g_ in gset:
                        for b in self.groups[g_]:
                            for d in b.deps:
                                if d.guard in gset:
                                    continue
                                sm, v = d.done
                                if ext.get(id(sm), (None, 0))[1] < v:
                                    ext[id(sm)] = (sm, v)
                    self.ext_cache[ck] = ext
                ext = self.ext_cache[ck]
                allb = [b for _, blk in chain for b in blk]
                if any(not b.dma for b in allb):
                    eng.drain()
                for k_, (sm, v) in ext.items():
                    if known.get(k_, 0) >= v:
                        continue
                    eng.wait_ge(sm, v)
                incs = {}
                for b in allb:
                    sm = b.done[0]
                    n_ = incs.get(id(sm), (sm, 0))[1]
                    incs[id(sm)] = (sm, n_ + (16 if b.dma else 1))
                for k_, (sm, n_) in incs.items():
                    eng.sem_inc(sm, n_)

        ops = self.ops[eng_name]
        for g_ in sorted({o.guard[1] for o in ops if o.guard is not None}):
            thr_reg(g_)
        i = 0
        while i < len(ops):
            o = ops[i]
            if o.guard is None:
                emit_one(o, known)
                i += 1
                continue
            chain = []
            j = i
            seen = set()
            while j < len(ops) and ops[j].guard is not None and ops[j].guard[0] == o.guard[0]:
                gd = ops[j].guard
                assert gd not in seen, ("guard re-appears non-contiguously", gd)
                assert not chain or gd[1] > chain[-1][0][1]
                seen.add(gd)
                k = j
                while k < len(ops) and ops[k].guard == gd:
                    k += 1
                chain.append((gd, ops[j:k]))
                j = k
            emit_chain(chain, known)
            i = j


class Arena:
    def __init__(self, ap, nbytes):
        self.ap = ap
        self.nbytes = nbytes
        self.off = 0
        self.hi = 0

    def alloc(self, shape, dtype, parts=128):
        esz = {F32: 4, BF16: 2, U8: 1, I32: 4, U32: 4}[dtype]
        n = int(np.prod(shape))
        nb = (n * esz + 63) // 64 * 64
        assert self.off + nb <= self.nbytes, ("arena overflow", self.off, nb, self.nbytes)
        v = self.ap[0:parts, self.off:self.off + n * esz]
        if dtype != U8:
            v = v.bitcast(dtype)
        if len(shape) == 2:
            v = v.rearrange("p (a b) -> p a b", a=shape[0])
        elif len(shape) == 3:
            v = v.rearrange("p (a b c) -> p a b c", a=shape[0], b=shape[1])
        self.off += nb
        self.hi = max(self.hi, self.off)
        return v

    def mark(self):
        return self.off

    def release(self, m):
        self.off = m


V_BADA = 0
V_GPRE = 48
V_LBR = 72
V_HNW = 88
V_GNW = 89
V_GKB = 90
V_BG = 94
V_BU = 350
NV = 606


def build_program(dbg=None):
    dbg = dbg or {}
    nc = bass.Bass("TRN2", target_bir_lowering=False)
    dt = nc.dram_tensor
    x_d = dt("x", [NSEQ * LAT, D], F32, kind="ExternalInput").ap()
    ctx_d = dt("ctx", [NSEQ * CTXL, D], F32, kind="ExternalInput").ap()
    cT_d = dt("cT", [128, 8, 3], F32, kind="ExternalInput").ap()
    vecs_d = dt("vecs", [128, NV], F32, kind="ExternalInput").ap()
    rowv_d = dt("rowv", [8, D], F32, kind="ExternalInput").ap()
    w_ada_d = dt("w_ada", [D, 6 * D], F32, kind="ExternalInput").ap()
    w_in_d = dt("w_in", [D, INW], F32, kind="ExternalInput").ap()
    w2_d = dt("gk_w2", [2, 16, 256], F32, kind="ExternalInput").ap()
    wupo_d = dt("wupo", [2048, D], F32, kind="ExternalInput").ap()
    wr_d = dt("w_router", [D, NEXP], F32, kind="ExternalInput").ap()
    wg_d = dt("w_gate", [NEXP, D, D], F32, kind="ExternalInput").ap()
    wu_d = dt("w_up", [NEXP, D, D], F32, kind="ExternalInput").ap()
    wd_d = dt("w_down", [NEXP, D, D], F32, kind="ExternalInput").ap()
    bd_d = dt("b_down", [NEXP, D], F32, kind="ExternalInput").ap()
    out_d = dt("out", [NSEQ * LAT, D], F32, kind="ExternalOutput").ap()
    winb_d = dt("winb", [D, INW], BF16, kind="Internal").ap()
    wupob_d = dt("wupob", [2048, D], BF16, kind="Internal").ap()
    bc_d = dt("bcrows", [NSEQ, 4, D], F32, kind="Internal").ap()
    x1_d = dt("x1s", [NSEQ * LAT, D], F32, kind="Internal").ap()
    dbg_out = {}
    for name, (shape, dtype) in dbg.get("outs", {}).items():
        dbg_out[name] = dt(name, list(shape), dtype, kind="ExternalOutput").ap()

    S = Sched()
    with ExitStack() as ctx:
        arena_t = ctx.enter_context(nc.sbuf_tensor("arena", [128, 206 * 1024], U8))
        A = Arena(arena_t, 206 * 1024)
        banks = [ctx.enter_context(nc.psum_tensor("pb%d" % i, [128, 512], F32)) for i in range(8)]
        PB = [Buf("pb%d" % i) for i in range(8)]

        def T(shape, dtype, name):
            return A.alloc(shape, dtype), Buf(name)

        identb, b_identb = T([128], BF16, "identb")
        identf, b_identf = T([128], F32, "identf")
        maskF, b_maskF = T([4, 128], BF16, "maskF")
        maskB, b_maskB = T([4, 128], BF16, "maskB")
        onesb, b_onesb = T([128], BF16, "onesb")
        onesf, b_onesf = T([128], F32, "onesf")
        m01, b_m01 = T([8, 128], F32, "m01")
        neghalf, b_neghalf = T([8], F32, "neghalf")
        vecs, b_vecs = T([NV], F32, "vecs")
        lbt, b_lbt = T([16], F32, "lbt")
        negb, b_negb = T([4], F32, "negb")
        w2s, b_w2s = T([2, 256], F32, "w2s")
        scT, b_scT = T([8, 3], F32, "scT")
        modT, b_modT = T([48, 3], F32, "modT")
        A1, b_A1 = T([3, 8], F32, "A1")
        tmpf, b_tmpf = T([128], F32, "tmpf")

        def c_memset(eng, ap, val, bufs):
            S.op(eng, lambda e: e.memset(ap, val), w=bufs)

        c_memset("pool", tmpf, 0.0, [b_tmpf])
        S.op("pool", lambda e: e.affine_select(out=identf, in_=tmpf, pattern=[[-1, 128]],
                                               compare_op=ALU.not_equal, fill=1.0, base=0,
                                               channel_multiplier=1), r=[b_tmpf], w=[b_identf])
        S.op("dve", lambda e: e.tensor_copy(out=identb, in_=identf), r=[b_identf], w=[b_identb])
        c_memset("pool", onesf, 1.0, [b_onesf])
        c_memset("pool", onesb, 1.0, [b_onesb])
        c_memset("pool", neghalf, -0.5, [b_neghalf])
        c_memset("pool", m01, 1.0, [b_m01])
        c_memset("pool", m01[:, :, 0:1], 0.0, [b_m01])
        ones4, b_ones4 = T([4, 128], F32, "ones4")
        c_memset("pool", ones4, 1.0, [b_ones4])
        mtmp, b_mtmp = T([4, 128], F32, "mtmp")
        S.op("pool", lambda e: e.affine_select(out=mtmp, in_=ones4, pattern=[[0, 4], [1, 128]],
                                               compare_op=ALU.is_ge, fill=0.0, base=0,
                                               channel_multiplier=-1), r=[b_ones4], w=[b_mtmp])
        S.op("dve", lambda e: e.tensor_copy(out=maskF, in_=mtmp), r=[b_mtmp], w=[b_maskF])
        S.op("pool", lambda e: e.affine_select(out=mtmp, in_=ones4, pattern=[[0, 4], [-1, 128]],
                                               compare_op=ALU.is_ge, fill=0.0, base=0,
                                               channel_multiplier=1), r=[b_ones4], w=[b_mtmp])
        S.op("dve", lambda e: e.tensor_copy(out=maskB, in_=mtmp), r=[b_mtmp], w=[b_maskB])

        S.op("sp", lambda e: e.dma_start(out=vecs, in_=vecs_d), w=[b_vecs], dma=True)
        S.op("sp", lambda e: e.dma_start(out=scT, in_=cT_d), w=[b_scT], dma=True)
        S.op("sp", lambda e: e.dma_start(out=w2s[0:16], in_=w2_d.rearrange("d r c -> r d c")),
             w=[b_w2s], dma=True)
        S.op("dve", lambda e: e.tensor_tensor(out=lbt[:, 0:8], in0=vecs[:, V_LBR:V_LBR + 8],
                                              in1=vecs[:, V_LBR + 8:V_LBR + 16], op=ALU.subtract),
             r=[b_vecs], w=[b_lbt])
        S.op("act", lambda e: e.activation(out=lbt[:, 0:8], in_=lbt[:, 0:8], func=AF.Sigmoid),
             r=[b_lbt], w=[b_lbt])
        S.op("dve", lambda e: e.tensor_scalar(out=lbt[:, 8:16], in0=lbt[:, 0:8], scalar1=-1.0, scalar2=1.0,
                                              op0=ALU.mult, op1=ALU.add), r=[b_lbt], w=[b_lbt])
        S.op("dve", lambda e: e.tensor_scalar(out=negb, in0=vecs[:, V_GKB:V_GKB + 4], scalar1=-1.0,
                                              scalar2=None, op0=ALU.mult), r=[b_vecs], w=[b_negb])
        S.op("act", lambda e: e.activation(out=scT, in_=scT, func=AF.Silu), r=[b_scT], w=[b_scT])

        b_winb = Buf("winb")
        b_wupob = Buf("wupob")
        for i in range(4):
            c0 = i * 1544
            S.op("pool", lambda e, c0=c0: e.dma_start(out=winb_d[:, c0:c0 + 1544], in_=w_in_d[:, c0:c0 + 1544]),
                 w=[b_winb], dma=True)
        S.op("pool", lambda e: e.dma_start(out=wupob_d, in_=wupo_d), w=[b_wupob], dma=True)

        m0 = A.mark()
        wab = [T([8, 512], F32, "wab%d" % i) for i in range(2)]
        rep = [T([8, 128], F32, "rep%d" % j) for j in range(NSEQ)]
        bct, b_bct = T([512], F32, "bct")
        bcs, b_bcs = T([512], F32, "bcs")
        for j in range(NSEQ):
            for kc in range(8):
                S.op("act", lambda e, j=j, kc=kc: e.activation(out=rep[j][0][:, kc, :], in_=onesf, func=AF.Identity,
                                                               scale=scT[:, kc, j:j + 1]),
                     r=[b_onesf, b_scT], w=[rep[j][1]])
        b_bcd = Buf("bc_d")
        bcseg = {4: 0, 5: 0, 6: 1, 7: 1, 8: 2, 9: 2, 10: 3, 11: 3}
        for blk in range(12):
            wt, wb_ = wab[blk % 2]
            S.op("sp", lambda e, wt=wt, blk=blk: e.dma_start(
                out=wt, in_=w_ada_d[:, blk * 512:(blk + 1) * 512].rearrange("(k p) n -> p k n", p=128)),
                w=[wb_], dma=True)
            def fm(e, wt=wt, blk=blk):
                ins = None
                for m in range(4):
                    for kc in range(8):
                        ins = e.matmul(banks[0][:, (blk * 4 + m) * 3:(blk * 4 + m) * 3 + 3],
                                       lhsT=wt[:, kc, m * 128:(m + 1) * 128], rhs=scT[:, kc, :],
                                       start=(kc == 0), stop=(kc == 7))
                return ins
            S.op("pe", fm, r=[wb_, b_scT], w=[PB[0]])
            if blk in bcseg:
                for j in range(NSEQ):
                    pbk = 1 + j
                    def bc(e, wt=wt, j=j, pbk=pbk):
                        ins = None
                        for kc in range(8):
                            ins = e.matmul(banks[pbk][:, :], lhsT=rep[j][0][:, kc, :], rhs=wt[:, kc, :],
                                           start=(kc == 0), stop=(kc == 7))
                        return ins
                    S.op("pe", bc, r=[wb_, rep[j][1]], w=[PB[pbk]])
                    row = bcseg[blk]
                    half = blk % 2
                    S.op("sp", lambda e, row=row, half=half: e.dma_start(
                        out=bct, in_=rowv_d[row:row + 1, half * 512:(half + 1) * 512].to_broadcast([128, 512])),
                        w=[b_bct], dma=True)
                    S.op("dve", lambda e, pbk=pbk: e.tensor_tensor(out=bcs, in0=banks[pbk][:, :], in1=bct, op=ALU.add),
                         r=[PB[pbk], b_bct], w=[b_bcs])
                    if row == 2:
                        S.op("dve", lambda e: e.tensor_scalar(out=bcs, in0=bcs, scalar1=1.0, scalar2=None, op0=ALU.add),
                             r=[b_bcs], w=[b_bcs])
                    grow = {0: 4, 2: 5, 3: 6}.get(row)
                    if grow is not None:
                        S.op("sp", lambda e, grow=grow, half=half: e.dma_start(
                            out=bct, in_=rowv_d[grow:grow + 1, half * 512:(half + 1) * 512].to_broadcast([128, 512])),
                            w=[b_bct], dma=True)
                        S.op("dve", lambda e: e.tensor_tensor(out=bcs, in0=bcs, in1=bct, op=ALU.mult),
                             r=[b_bcs, b_bct], w=[b_bcs])
                    S.op("sp", lambda e, j=j, row=row, half=half: e.dma_start(
                        out=bc_d[j, row:row + 1, half * 512:(half + 1) * 512], in_=bcs[0:1, :]),
                        r=[b_bcs], w=[b_bcd], dma=True)
        S.op("dve", lambda e: e.tensor_tensor(
            out=modT, in0=banks[0][:, 0:144].rearrange("p (c j) -> p c j", j=3),
            in1=vecs[:, V_BADA:V_BADA + 48].unsqueeze(2).to_broadcast([128, 48, 3]), op=ALU.add),
            r=[PB[0], b_vecs], w=[b_modT])
        for j in range(3):
            S.op("dve", lambda e, j=j: e.scalar_tensor_tensor(
                out=A1[:, j, :], in0=modT[:, 8:16, j], scalar=1.0, in1=vecs[:, V_GPRE:V_GPRE + 8],
                op0=ALU.add, op1=ALU.mult), r=[b_modT, b_vecs], w=[b_A1])
        A.release(m0)
        S.fence()

        if "modT" in dbg_out:
            S.op("sp", lambda e: e.dma_start(out=dbg_out["modT"], in_=modT), r=[b_modT], w=[Buf("o")], dma=True)
        if "A1" in dbg_out:
            S.op("sp", lambda e: e.dma_start(out=dbg_out["A1"], in_=A1), r=[b_A1], w=[Buf("o")], dma=True)
        if "lbt" in dbg_out:
            S.op("sp", lambda e: e.dma_start(out=dbg_out["lbt"], in_=lbt), r=[b_lbt], w=[Buf("o")], dma=True)
        if "bc" in dbg_out:
            S.op("sp", lambda e: e.dma_start(out=dbg_out["bc"], in_=bc_d), r=[b_bcd], w=[Buf("o")], dma=True)

        def dump(name, ap, bufs):
            if name in dbg_out:
                S.op("sp", lambda e: e.dma_start(out=dbg_out[name], in_=ap), r=bufs, w=[Buf("o")], dma=True)

        CAP = dbg.get("cap", 2048)
        GR = 256
        NGR = CAP // GR
        NROWS = NEXP * CAP
        xg_d = dt("xg", [NROWS, D], BF16, kind="Internal").ap()
        yg_d = dt("yg", [NROWS, D], F32, kind="Internal").ap()
        b_xg = Buf("xg")
        b_yg = Buf("yg")
        NTT = NSEQ * LAT // 128
        P_all, b_Pall = T([NTT, NEXP], F32, "P_all")
        pk_all, b_pkall = T([NTT, 4], F32, "pk_all")
        desti, b_desti = T([NTT, 4], I32, "desti")
        basec, b_basec = T([NEXP], F32, "basec")
        cnti, b_cnti = T([NEXP], I32, "cnti")
        ecap, b_ecap = T([NEXP], F32, "ecap")
        ecapi, b_ecapi = T([NEXP], I32, "ecapi")
        ustr, b_ustr = T([128], BF16, "ustr")
        brt, b_brt = T([NEXP], F32, "brt")
        wr, b_wr = T([8, NEXP], F32, "wr")
        S.op("pool", lambda e: e.memset(basec, 0.0), w=[b_basec])
        S.op("pool", lambda e: e.iota(ecapi, pattern=[[CAP, NEXP]], base=0, channel_multiplier=0), w=[b_ecapi])
        S.op("dve", lambda e: e.tensor_copy(out=ecap, in_=ecapi), r=[b_ecapi], w=[b_ecap])
        S.op("pool", lambda e: e.affine_select(out=tmpf, in_=onesf, pattern=[[1, 128]], compare_op=ALU.is_gt, fill=0.0,
                                               base=0, channel_multiplier=-1), r=[b_onesf], w=[b_tmpf])
        S.op("dve", lambda e: e.tensor_copy(out=ustr, in_=tmpf), r=[b_tmpf], w=[b_ustr])
        S.op("sp", lambda e: e.dma_start(out=brt, in_=rowv_d[7:8, 0:NEXP].to_broadcast([128, NEXP])), w=[b_brt], dma=True)
        S.op("sp", lambda e: e.dma_start(out=wr, in_=wr_d.rearrange("(k p) n -> p k n", p=128)), w=[b_wr], dma=True)
        mix_mark = A.mark()
        NW = 3
        wbuf = [T([8, 512], BF16, "wbuf%d" % i) for i in range(NW)]
        wctr = [0]

        def wload(src_ap, view=None):
            wt, wb_ = wbuf[wctr[0] % NW]
            wctr[0] += 1
            dst = view(wt) if view is not None else wt
            S.op("sp", lambda e: e.dma_start(out=dst, in_=src_ap), r=[b_winb, b_wupob], w=[wb_], dma=True)
            return wt, wb_

        pjc = [0]

        def pjbank():
            i = 1 + (pjc[0] % 5)
            pjc[0] += 1
            return banks[i], PB[i]

        xt = [T([1024], F32, "xt%d" % i) for i in range(2)]
        xnb = [T([1024], BF16, "xnb%d" % i) for i in range(2)]
        hT, b_hT = T([8, ST], BF16, "hT")
        ss, b_ss = T([8], F32, "ss")
        b_ssr = Buf("ssr")
        b_st2r = Buf("st2r")
        junk, b_junk = T([1024], BF16, "junk")
        qA, b_qA = T([4, ST], BF16, "qA")
        fA, b_fA = T([4, ST], F32, "fA")
        lfA, b_lfA = T([4, ST], F32, "lfA")
        preA, b_preA = T([4, ST], F32, "preA")
        EpA, b_EpA = T([4, ST], BF16, "EpA")
        EmA, b_EmA = T([4, ST], BF16, "EmA")
        qeA = [T([4, ST], BF16, "qeA%d" % d) for d in range(2)]
        keA = [T([4, ST], BF16, "keA%d" % d) for d in range(2)]
        aA = [T([4, 2], F32, "aA%d" % d) for d in range(2)]
        rT, b_rT = T([2, ST], F32, "rT")
        qB, b_qB = T([2, ST], BF16, "qB")
        kB, b_kB = T([2, ST], BF16, "kB")
        sB, b_sB = T([2, ST], F32, "sB")
        preB, b_preB = T([2, ST], F32, "preB")
        EpB, b_EpB = T([2, ST], BF16, "EpB")
        EmB, b_EmB = T([2, ST], BF16, "EmB")
        qeB = [T([2, 2, ST], BF16, "qeB%d" % d) for d in range(2)]
        pm, b_pm = T([2], F32, "pm")
        S.op("pool", lambda e: e.memset(pm, 0.0), w=[b_pm])
        S.op("pool", lambda e: e.memset(pm[0:64, 0:1], 1.0), w=[b_pm])
        S.op("pool", lambda e: e.memset(pm[64:128, 1:2], 1.0), w=[b_pm])
        keB = [T([2, ST], BF16, "keB%d" % d) for d in range(2)]
        aB = [T([2, 2], F32, "aB%d" % d) for d in range(2)]
        vA, b_vA = T([2, 512], BF16, "vA")
        vB, b_vB = T([2, 512], BF16, "vB")
        sgA, b_sgA = T([4, ST], BF16, "sgA")
        sgB, b_sgB = T([4, ST], BF16, "sgB")
        smA, b_smA = T([8, ST], BF16, "smA")
        smB, b_smB = T([8, ST], BF16, "smB")
        kdA, b_kdA = T([4, 128], BF16, "kdA")
        kdB, b_kdB = T([2, 128], BF16, "kdB")
        WA, b_WA = T([4, 128], F32, "WA")
        WB, b_WB = T([2, 128], F32, "WB")
        VA, b_VA = T([4, 128], F32, "VA")
        VB, b_VB = T([2, 128], F32, "VB")
        tsA, b_tsA = T([4, 128], F32, "tsA")
        tsB, b_tsB = T([2, 128], F32, "tsB")
        SfA, b_SfA = T([16, 4, 128], BF16, "SfA")
        SfB, b_SfB = T([16, 2, 128], BF16, "SfB")
        SbA, b_SbA = T([4, 128], BF16, "SbA")
        SbB, b_SbB = T([2, 128], BF16, "SbB")
        scF, b_scF = T([4, 128], BF16, "scF")
        scBt, b_scBt = T([4, 128], BF16, "scBt")
        oTs, b_oTs = T([4, 128], F32, "oTs")
        sq, b_sq = T([4, 128], BF16, "sq")
        rstd, b_rstd = T([4, 128], F32, "rstd")
        rdA, b_rdA = T([4, ST], BF16, "rdA")
        rdB, b_rdB = T([4, ST], BF16, "rdB")
        mrgT, b_mrgT = T([8, ST], BF16, "mrgT")
        mt1, b_mt1 = T([ST], F32, "mt1")
        mt2, b_mt2 = T([ST], F32, "mt2")
        mixs, b_mixs = T([1024], F32, "mixs")
        xres, b_xres = T([1024], F32, "xres")
        x1t, b_x1t = T([1024], F32, "x1t")
        G1, b_G1 = T([1024], F32, "G1")
        st2, b_st2 = T([8], F32, "st2")
        A2, b_A2 = T([1024], F32, "A2")
        SH2, b_SH2 = T([1024], F32, "SH2")
        h2b, b_h2b = T([1024], BF16, "h2b")
        lg, b_lg = T([NEXP], F32, "lg")
        mx8, b_mx8 = T([8], F32, "mx8")
        msk, b_msk = T([NEXP], F32, "msk")
        mskb, b_mskb = T([NEXP], BF16, "mskb")
        exq, b_exq = T([NEXP], F32, "exq")
        slot, b_slot = T([NEXP], F32, "slot")
        oh, b_oh = T([NEXP], F32, "oh")
        j32, b_j32 = T([NEXP], F32, "j32")
        destf, b_destf = T([4], F32, "destf")
        sm1, b_sm1 = T([4], F32, "sm1")

        breg = {}

        def bcreg(e):
            if "r" not in breg:
                breg["r"] = e.to_reg(NROWS - 1)
            return breg["r"]

        def m1_tile(gt):
            h2T = xres.rearrange("p (k t) -> p k t", k=8)
            S.op("act", lambda e: e.activation(out=junk, in_=x1t, func=AF.Square, accum_out=st2[:, 5:6]),
                 r=[b_x1t], w=[b_junk, b_st2r])
            S.op("act", lambda e: e.activation(out=st2[:, 1:2], in_=st2[:, 5:6], func=AF.Copy),
                 r=[b_st2r], w=[b_st2])
            S.op("pool", lambda e: e.tensor_scalar(out=st2[:, 1:2], in0=st2[:, 1:2], scalar1=1.0 / D, scalar2=EPS,
                                                   op0=ALU.mult, op1=ALU.add), r=[b_st2], w=[b_st2])
            S.op("pool", lambda e: e.tensor_tensor(out=st2[:, 1:2], in0=st2[:, 1:2], in1=neghalf[:, 0:1], op=ALU.pow),
                 r=[b_st2, b_neghalf], w=[b_st2])
            S.op("dve", lambda e: e.scalar_tensor_tensor(out=mixs, in0=x1t, scalar=st2[:, 1:2], in1=A2,
                                                         op0=ALU.mult, op1=ALU.mult),
                 r=[b_x1t, b_st2, b_A2], w=[b_mixs])
            S.op("dve", lambda e: e.tensor_tensor(out=mixs, in0=mixs, in1=SH2, op=ALU.add),
                 r=[b_mixs, b_SH2], w=[b_mixs])
            S.op("act", lambda e: e.activation(out=h2b, in_=mixs, func=AF.Copy), r=[b_mixs], w=[b_h2b])
            dump("h2b_%d" % gt, h2b, [b_h2b])
            for hf in range(2):
                bk, bb = pjbank()

                def tr(e, hf=hf, bk=bk):
                    ins = None
                    for q in range(4):
                        kc = hf * 4 + q
                        ins = e.transpose(out=bk[:, q * 128:(q + 1) * 128], in_=mixs[:, kc * 128:(kc + 1) * 128],
                                          identity=identf)
                    return ins
                S.op("pe", tr, r=[b_mixs, b_identf], w=[bb])
                S.op("act", lambda e, hf=hf, bk=bk: e.activation(
                    out=xres[:, hf * 512:(hf + 1) * 512], in_=bk[:, :], func=AF.Copy), r=[bb], w=[b_xres])
            bk, bb = pjbank()

            def lgm(e, bk=bk):
                ins = None
                for kc in range(8):
                    ins = e.matmul(bk[:, 0:NEXP], lhsT=h2T[:, kc, :], rhs=wr[:, kc, :], start=(kc == 0), stop=(kc == 7))
                return ins
            S.op("pe", lgm, r=[b_xres, b_wr], w=[bb])
            S.op("dve", lambda e, bk=bk: e.tensor_tensor(out=lg, in0=bk[:, 0:NEXP], in1=brt, op=ALU.add),
                 r=[bb, b_brt], w=[b_lg])
            dump("lg_%d" % gt, lg, [b_lg])
            S.op("dve", lambda e: e.max(out=mx8, in_=lg), r=[b_lg], w=[b_mx8])
            S.op("dve", lambda e: e.tensor_scalar(out=sm1[:, 0:1], in0=mx8[:, 0:1], scalar1=-1.0, scalar2=None,
                                                  op0=ALU.mult), r=[b_mx8], w=[b_sm1])
            S.op("dve", lambda e: e.tensor_scalar(out=msk, in0=lg, scalar1=mx8[:, 3:4], scalar2=None, op0=ALU.is_ge),
                 r=[b_lg, b_mx8], w=[b_msk])
            S.op("pool", lambda e: e.tensor_copy(out=mskb, in_=msk), r=[b_msk], w=[b_mskb])
            S.op("act", lambda e: e.activation(out=exq, in_=lg, func=AF.Exp, bias=sm1[:, 0:1]),
                 r=[b_lg, b_sm1], w=[b_exq])
            S.op("dve", lambda e: e.scalar_tensor_tensor(out=exq, in0=exq, scalar=1.0, in1=msk,
                                                         op0=ALU.mult, op1=ALU.mult, accum_out=sm1[:, 1:2]),
                 r=[b_exq, b_msk], w=[b_exq, b_sm1])
            S.op("dve", lambda e: e.reciprocal(out=sm1[:, 2:3], in_=sm1[:, 1:2]), r=[b_sm1], w=[b_sm1])
            S.op("dve", lambda e: e.tensor_scalar(out=P_all[:, gt, :], in0=exq, scalar1=sm1[:, 2:3], scalar2=None,
                                                  op0=ALU.mult), r=[b_exq, b_sm1], w=[b_Pall])
            bk2, bb2 = pjbank()

            def posm(e, bk2=bk2):
                e.matmul(bk2[:, 0:NEXP], lhsT=ustr, rhs=mskb, start=True, stop=True)
                return e.matmul(bk2[:, NEXP:2 * NEXP], lhsT=onesb, rhs=mskb, start=True, stop=True)
            S.op("pe", posm, r=[b_ustr, b_onesb, b_mskb], w=[bb2])
            S.op("dve", lambda e, bk2=bk2: e.tensor_tensor(out=slot, in0=bk2[:, 0:NEXP], in1=basec, op=ALU.add),
                 r=[bb2, b_basec], w=[b_slot])
            S.op("dve", lambda e: e.tensor_scalar(out=slot, in0=slot, scalar1=float(CAP - 1), scalar2=None, op0=ALU.min),
                 r=[b_slot], w=[b_slot])
            S.op("dve", lambda e: e.tensor_tensor(out=slot, in0=slot, in1=ecap, op=ALU.add),
                 r=[b_slot, b_ecap], w=[b_slot])
            S.op("dve", lambda e, bk2=bk2: e.tensor_tensor(out=basec, in0=bk2[:, NEXP:2 * NEXP], in1=basec, op=ALU.add),
                 r=[bb2, b_basec], w=[b_basec])
            for k in range(4):
                S.op("dve", lambda e, k=k: e.tensor_scalar(out=oh, in0=lg, scalar1=mx8[:, k:k + 1], scalar2=None,
                                                           op0=ALU.is_equal), r=[b_lg, b_mx8], w=[b_oh])
                S.op("dve", lambda e, k=k: e.scalar_tensor_tensor(out=j32, in0=oh, scalar=1.0, in1=slot,
                                                                  op0=ALU.mult, op1=ALU.mult, accum_out=destf[:, k:k + 1]),
                     r=[b_oh, b_slot], w=[b_j32, b_destf])
                S.op("dve", lambda e, k=k: e.scalar_tensor_tensor(out=j32, in0=oh, scalar=1.0, in1=P_all[:, gt, :],
                                                                  op0=ALU.mult, op1=ALU.mult,
                                                                  accum_out=pk_all[:, gt, k:k + 1]),
                     r=[b_oh, b_Pall], w=[b_j32, b_pkall])
            S.op("dve", lambda e: e.tensor_copy(out=desti[:, gt, :], in_=destf), r=[b_destf], w=[b_desti])
            for k in range(4):
                S.op("pool", lambda e, k=k: e.indirect_dma_start(
                    out=xg_d[:, :], out_offset=bass.IndirectOffsetOnAxis(ap=desti[:, gt, k:k + 1], axis=0),
                    in_=h2b[:, :], in_offset=None, bounds_check=bcreg(e), oob_is_err=False),
                    r=[b_h2b, b_desti], w=[b_xg], dma=True)

        MIX = {
            "A": dict(H=4, q=(qA, b_qA), pre=(preA, b_preA), lf=(lfA, b_lfA), Ep=(EpA, b_EpA), Em=(EmA, b_EmA),
                      qe=qeA, ke=keA, a=aA, v=(vA, b_vA), kd=(kdA, b_kdA), W=(WA, b_WA), V=(VA, b_VA),
                      ts=(tsA, b_tsA), Sf=(SfA, b_SfA), Sb=(SbA, b_SbA), sg=(sgA, b_sgA), rd=(rdA, b_rdA),
                      sgn=1.0, nw=V_HNW),
            "B": dict(H=2, q=(qB, b_qB), pre=(preB, b_preB), lf=(sB, b_sB), Ep=(EpB, b_EpB), Em=(EmB, b_EmB),
                      qe=qeB, ke=keB, a=aB, v=(vB, b_vB), kd=(kdB, b_kdB), W=(WB, b_WB), V=(VB, b_VB),
                      ts=(tsB, b_tsB), Sf=(SfB, b_SfB), Sb=(SbB, b_SbB), sg=(sgB, b_sgB), rd=(rdB, b_rdB),
                      sgn=-1.0 / 16.0, nw=V_GNW),
        }

        def load_norm_transpose(src_rows, j):
            for t in range(2):
                xa, xb_ = xt[t]
                na, nb_ = xnb[t]
                S.op("sp", lambda e, xa=xa, t=t: e.dma_start(out=xa, in_=src_rows[t * 128:(t + 1) * 128, :]),
                     w=[xb_], dma=True)
                S.op("act", lambda e, xa=xa, t=t: e.activation(out=junk, in_=xa, func=AF.Square,
                                                                accum_out=ss[:, 4 + t:5 + t]),
                     r=[xb_], w=[b_junk, b_ssr])
                S.op("act", lambda e, t=t: e.activation(out=ss[:, t:t + 1], in_=ss[:, 4 + t:5 + t], func=AF.Copy),
                     r=[b_ssr], w=[b_ss])
                S.op("pool", lambda e, t=t: e.tensor_scalar(out=ss[:, t:t + 1], in0=ss[:, t:t + 1], scalar1=1.0 / D,
                                                            scalar2=EPS, op0=ALU.mult, op1=ALU.add),
                     r=[b_ss], w=[b_ss])
                S.op("pool", lambda e, t=t: e.tensor_tensor(out=ss[:, t:t + 1], in0=ss[:, t:t + 1],
                                                            in1=neghalf[:, 0:1], op=ALU.pow),
                     r=[b_ss, b_neghalf], w=[b_ss])
                S.op("dve", lambda e, xa=xa, na=na, t=t: e.tensor_scalar(out=na, in0=xa, scalar1=ss[:, t:t + 1],
                                                                        scalar2=None, op0=ALU.mult),
                     r=[xb_, b_ss], w=[nb_])
                tpv = banks[0][:, :].bitcast(BF16).rearrange("p (k n) -> p k n", k=8)

                def tr(e, na=na):
                    ins = None
                    for kc in range(8):
                        ins = e.transpose(out=tpv[:, kc, :], in_=na[:, kc * 128:(kc + 1) * 128], identity=identb)
                    return ins
                S.op("pe", tr, r=[nb_, b_identb], w=[PB[0]])
                for kc in range(8):
                    S.op("act", lambda e, kc=kc, t=t: e.activation(
                        out=hT[:, kc, t * 128:(t + 1) * 128], in_=tpv[:, kc, :], func=AF.Identity,
                        scale=A1[:, j, kc:kc + 1], bias=modT[:, kc, j:j + 1]),
                        r=[PB[0], b_A1, b_modT], w=[b_hT])

        def proj_fm(c0, ncols, msize, consumer, mlist=None):
            wt, wb_ = wload(winb_d[:, c0:c0 + ncols].rearrange("(k p) n -> p k n", p=128),
                            view=lambda w: w[:, :, 0:ncols])
            nm = ncols // msize
            for m in (mlist if mlist is not None else range(nm)):
                bk, bb = pjbank()

                def mm(e, m=m, bk=bk):
                    ins = None
                    for kc in range(8):
                        ins = e.matmul(bk[0:msize, 0:ST], lhsT=wt[:, kc, m * msize:(m + 1) * msize],
                                       rhs=hT[:, kc, :], start=(kc == 0), stop=(kc == 7))
                    return ins
                S.op("pe", mm, r=[wb_, b_hT], w=[bb])
                consumer(m, bk, bb)

        def proj_tm(c0, dst, b_dst):
            wt, wb_ = wload(winb_d[:, c0:c0 + 512].rearrange("(k p) n -> p k n", p=128))
            for t in range(2):
                bk, bb = pjbank()

                def mm(e, t=t, bk=bk):
                    ins = None
                    for kc in range(8):
                        ins = e.matmul(bk[:, :], lhsT=hT[:, kc, t * 128:(t + 1) * 128], rhs=wt[:, kc, :],
                                       start=(kc == 0), stop=(kc == 7))
                    return ins
                S.op("pe", mm, r=[wb_, b_hT], w=[bb])
                S.op("act", lambda e, t=t, bk=bk: e.activation(out=dst[:, t, :], in_=bk[:, :], func=AF.Copy),
                     r=[bb], w=[b_dst])

        def act_evac(dst, b_dst, func, scale=1.0):
            def c(m, bk, bb):
                S.op("act", lambda e: e.activation(out=dst[:, m, :], in_=bk[:, 0:ST], func=func, scale=scale),
                     r=[bb], w=[b_dst])
            return c

        def gates_gen(X, d, need_q):
            M = MIX[X]
            H = M["H"]
            pre, b_pre = M["pre"]
            lf, b_lf = M["lf"]
            Ep, b_Ep = M["Ep"]
            Em, b_Em = M["Em"]
            a, b_a = M["a"][d]
            sgn = M["sgn"]
            pre2 = pre.rearrange("p h t -> p (h t)")
            lf2 = lf.rearrange("p h t -> p (h t)")
            m2 = m01.rearrange("p h t -> p (h t)")[:, 0:H * ST]
            S.op("dve", lambda e: e.tensor_tensor_scan(out=pre2, data0=m2, data1=lf2, initial=0.0,
                                                       op0=ALU.mult, op1=ALU.add),
                 r=[b_lf, b_m01], w=[b_pre])
            yield
            totv = pre.rearrange("p h (c t) -> p h c t", c=2)[:, :, :, 127]
            S.op("act", lambda e: e.activation(out=a, in_=totv, func=AF.Exp, scale=sgn), r=[b_pre], w=[b_a])
            if d == 0:
                sp_, sm_ = sgn, -sgn
            else:
                S.op("dve", lambda e: e.tensor_tensor(out=pre2, in0=pre2, in1=lf2, op=ALU.subtract),
                     r=[b_pre, b_lf], w=[b_pre])
                sp_, sm_ = -sgn, sgn
            S.op("act", lambda e: e.activation(out=Em, in_=pre, func=AF.Exp, scale=sm_), r=[b_pre], w=[b_Em])
            ke, b_ke = M["ke"][d]
            if X == "A":
                S.op("dve", lambda e: e.scalar_tensor_tensor(out=ke, in0=fA, scalar=1.0, in1=Em, op0=ALU.subtract,
                                                             op1=ALU.mult), r=[b_fA, b_Em], w=[b_ke])
            else:
                S.op("dve", lambda e: e.tensor_tensor(out=ke, in0=kB, in1=Em, op=ALU.mult),
                     r=[b_kB, b_Em], w=[b_ke])
            yield
            qmode = dbg.get("qmode", 2)
            if need_q and qmode >= 1 and X in dbg.get("qmix", ("A", "B")):
                S.op("act", lambda e: e.activation(out=Ep, in_=pre, func=AF.Exp, scale=sp_), r=[b_pre], w=[b_Ep])
                q, b_q = M["q"]
                qe, b_qe = M["qe"][d]
                if qmode >= 2 and X == "A":
                    S.op("dve", lambda e: e.tensor_tensor(out=qe, in0=q, in1=Ep, op=ALU.mult),
                         r=[b_q, b_Ep], w=[b_qe])
                elif qmode >= 2:
                    for par in range(2):
                        S.op("dve", lambda e, par=par: e.scalar_tensor_tensor(
                            out=qe[:, par], in0=Ep, scalar=pm[:, par:par + 1], in1=q, op0=ALU.mult, op1=ALU.mult),
                            r=[b_q, b_Ep, b_pm], w=[b_qe])

        def gates(X, d, need_q):
            for _ in gates_gen(X, d, need_q):
                pass

        def zproj_gen(d, need_q, last_hT_use=False):
            c0 = 512 + 512 * d
            proj_fm(c0, 512, 128, act_evac(fA, b_fA, AF.Sigmoid))
            if last_hT_use:
                run_prefetch()
            for h in range(4):
                S.op("dve", lambda e, h=h: e.tensor_scalar(out=fA[:, h, :], in0=fA[:, h, :],
                                                           scalar1=lbt[:, 8 + d * 4 + h:8 + d * 4 + h + 1],
                                                           scalar2=lbt[:, d * 4 + h:d * 4 + h + 1],
                                                           op0=ALU.mult, op1=ALU.add),
                     r=[b_fA, b_lbt], w=[b_fA])
            yield
            S.op("act", lambda e: e.activation(out=lfA, in_=fA, func=AF.Ln), r=[b_fA], w=[b_lfA])
            yield
            yield from gates_gen("A", d, need_q)

        def zproj(d, need_q, last_hT_use=False):
            for _ in zproj_gen(d, need_q, last_hT_use):
                pass

        def rproj():
            wt, wb_ = wload(winb_d[:, 3584:3616].rearrange("(k p) n -> p k n", p=128), view=lambda w: w[:, :, 0:32])
            bk, bb = pjbank()

            def mm(e):
                ins = None
                for d in range(2):
                    for kc in range(8):
                        ins = e.matmul(bk[0:16, d * ST:(d + 1) * ST], lhsT=wt[:, kc, d * 16:(d + 1) * 16],
                                       rhs=hT[:, kc, :], start=(kc == 0), stop=(kc == 7))
                return ins
            S.op("pe", mm, r=[wb_, b_hT], w=[bb])
            S.op("act", lambda e: e.activation(out=rT[0:16].rearrange("p d t -> p (d t)"), in_=bk[0:16, :],
                                               func=AF.Copy), r=[bb], w=[b_rT])

        def gproj_gen(d, need_q):
            bk, bb = pjbank()

            def mm(e):
                ins = None
                for kt in range(2):
                    ins = e.matmul(bk[:, kt * ST:(kt + 1) * ST], lhsT=w2s[0:16, d, kt * 128:(kt + 1) * 128],
                                   rhs=rT[0:16, d, :], start=True, stop=True)
                return ins
            S.op("pe", mm, r=[b_w2s, b_rT], w=[bb])
            for kt in range(2):
                S.op("act", lambda e, kt=kt: e.activation(out=sB[:, kt, :], in_=bk[:, kt * ST:(kt + 1) * ST],
                                                          func=AF.Exp, scale=-1.0,
                                                          bias=negb[:, d * 2 + kt:d * 2 + kt + 1]),
                     r=[bb, b_negb], w=[b_sB])
            S.op("act", lambda e: e.activation(out=sB, in_=sB, func=AF.Ln, bias=1.0), r=[b_sB], w=[b_sB])
            yield
            yield from gates_gen("B", d, need_q)

        def gproj(d, need_q):
            for _ in gproj_gen(d, need_q):
                pass

        def state_products(X, d, c, bankbuf):
            M = MIX[X]
            H = M["H"]
            ke, b_ke = M["ke"][d]
            kd, b_kd = M["kd"]
            v, b_v = M["v"]
            bk, bb = bankbuf
            tpv = bk[:, :].bitcast(BF16)

            def tr(e):
                ins = None
                for h in range(H):
                    ins = e.transpose(out=tpv[:, h * 128:(h + 1) * 128], in_=ke[:, h, c * 128:(c + 1) * 128],
                                      identity=identb)
                return ins
            S.op("pe", tr, r=[b_ke, b_identb], w=[bb])
            S.op("act", lambda e: e.activation(out=kd.rearrange("p h t -> p (h t)"), in_=tpv[:, 0:H * 128],
                                               func=AF.Copy), r=[bb], w=[b_kd])

            def mm(e):
                ins = None
                for h in range(4):
                    if X == "A":
                        ins = e.matmul(bk[:, h * 128:(h + 1) * 128], lhsT=kd[:, h, :],
                                       rhs=v[:, c, h * 128:(h + 1) * 128], start=True, stop=True)
                    else:
                        p0 = (h % 2) * 64
                        ins = e.matmul(bk[p0:p0 + 64, (h // 2) * 128:(h // 2 + 1) * 128],
                                       lhsT=kd[:, h // 2, p0:p0 + 64],
                                       rhs=v[:, c, h * 128:(h + 1) * 128], start=True, stop=True)
                return ins
            S.op("pe", mm, r=[b_kd, b_v], w=[bb])
            return bk[:, 0:H * 128].rearrange("p (h t) -> p h t", h=H), bb

        def fwd_update(X, d, c, store_idx):
            M = MIX[X]
            H = M["H"]
            W, b_W = M["W"]
            a, b_a = M["a"][d]
            Sf, b_Sf = M["Sf"]
            P, bb = state_products(X, d, c, (banks[6], PB[6]))
            S.op("dve", lambda e: e.tensor_tensor(out=W, in0=P, in1=W, op=ALU.add), r=[bb, b_W], w=[b_W])
            S.op("dve", lambda e: e.tensor_tensor(out=W, in0=W,
                                                  in1=a[:, :, c].unsqueeze(2).to_broadcast([128, H, 128]),
                                                  op=ALU.mult), r=[b_W, b_a], w=[b_W])
            if store_idx is not None:
                S.op("pool", lambda e: e.tensor_copy(out=Sf[:, store_idx], in_=W), r=[b_W], w=[b_Sf])

        def bwd_update(X, d, c, need_sb):
            M = MIX[X]
            H = M["H"]
            V, b_V = M["V"]
            ts, b_ts = M["ts"]
            a, b_a = M["a"][d]
            Sb, b_Sb = M["Sb"]
            S.op("dve", lambda e: e.tensor_tensor(out=ts, in0=V,
                                                  in1=a[:, :, c].unsqueeze(2).to_broadcast([128, H, 128]),
                                                  op=ALU.mult), r=[b_V, b_a], w=[b_ts])
            if need_sb:
                S.op("pool", lambda e: e.tensor_copy(out=Sb, in_=ts), r=[b_ts], w=[b_Sb])
            P, bb = state_products(X, d, c, (banks[6], PB[6]))
            S.op("dve", lambda e: e.tensor_tensor(out=V, in0=P, in1=ts, op=ALU.add), r=[bb, b_ts], w=[b_V])

        pf = {"fn": None}

        def run_prefetch():
            f_ = pf["fn"]
            pf["fn"] = None
            if f_ is not None:
                f_()

        def interleave(chains, fillers):
            fillers = list(fillers)
            for ch in chains:
                for _ in ch:
                    if fillers:
                        fillers.pop(0)()
            for f_ in fillers:
                f_()

        def st_common(src_rows, j, p2, need_b):
            if p2:
                proj_fm(0, 512, 128, act_evac(qA, b_qA, AF.Copy, scale=-1.0))
            proj_tm(1536, vA, b_vA)
            if p2:
                proj_fm(2560, 512, 128, lambda m, bk, bb: (act_evac(qB, b_qB, AF.Copy, scale=-0.125)(m, bk, bb) if m < 2
                                                          else act_evac(kB, b_kB, AF.Copy, scale=-1.0)(m - 2, bk, bb)))
            else:
                proj_fm(2560, 512, 128, lambda m, bk, bb: act_evac(kB, b_kB, AF.Copy, scale=-1.0)(m - 2, bk, bb),
                        mlist=[2, 3])
            proj_tm(3072, vB, b_vB)
            rproj()

        def pass1_st(seq, is_ctx, sti):
            rproj()
            fillers = [
                lambda: proj_fm(2560, 512, 128, lambda m, bk, bb: act_evac(kB, b_kB, AF.Copy, scale=-1.0)(m - 2, bk, bb),
                                mlist=[2, 3]),
                lambda: proj_tm(1536, vA, b_vA),
                lambda: proj_tm(3072, vB, b_vB),
            ]
            interleave([zproj_gen(0, False)], fillers)
            if not is_ctx:
                run_prefetch()

            def fw(X, c):
                g = (0 if is_ctx else 2 + sti * 2) + c
                store = g + 1 - 2 if (g + 1 >= 2 and g + 1 - 2 < 16) else None
                return lambda: fwd_update(X, 0, c, store)

            def bw(X, c):
                return lambda: bwd_update(X, 1, c, False)
            if is_ctx:
                interleave([zproj_gen(1, False)], [fw("A", 0), fw("A", 1)])
                run_prefetch()
                interleave([gproj_gen(0, False)], [bw("A", 1), bw("A", 0)])
                interleave([gproj_gen(1, False)], [fw("B", 0), fw("B", 1)])
                bw("B", 1)()
                bw("B", 0)()
            else:
                interleave([gproj_gen(0, False)], [fw("A", 0), fw("A", 1)])
                fw("B", 0)()
                fw("B", 1)()

        def pass2_st(seq, sti):
            stop = dbg.get("p2_stop", 99)
            rproj()

            def sm_filler(dst, b_dst, c0, half):
                return lambda: proj_fm(c0 + half * 512, 512, 128,
                                       lambda m, bk, bb: act_evac(dst, b_dst, AF.Sigmoid)(half * 4 + m, bk, bb))
            fillers = [
                lambda: proj_fm(0, 512, 128, act_evac(qA, b_qA, AF.Copy, scale=-1.0)),
                lambda: proj_fm(2560, 512, 128, lambda m, bk, bb: (
                    act_evac(qB, b_qB, AF.Copy, scale=-0.125)(m, bk, bb) if m < 2
                    else act_evac(kB, b_kB, AF.Copy, scale=-1.0)(m - 2, bk, bb))),
                lambda: proj_tm(1536, vA, b_vA),
                lambda: proj_tm(3072, vB, b_vB),
                lambda: proj_fm(2048, 512, 128, act_evac(sgA, b_sgA, AF.Silu)),
                lambda: proj_fm(3616, 512, 128, act_evac(sgB, b_sgB, AF.Silu)),
                sm_filler(smA, b_smA, 4128, 0), sm_filler(smA, b_smA, 4128, 1),
                sm_filler(smB, b_smB, 5152, 0), sm_filler(smB, b_smB, 5152, 1),
            ]
            fillers.append(run_prefetch)
            interleave([zproj_gen(0, True), zproj_gen(1, True), gproj_gen(0, True), gproj_gen(1, True)], fillers)
            run_prefetch()
            if stop <= 3:
                return
            for c in (1, 0):
                cl = sti * 2 + c
                cs = slice(c * 128, (c + 1) * 128)
                for X in dbg.get("p2_mixers", ("A", "B")):
                    M = MIX[X]
                    v, b_v = M["v"]
                    Sf, b_Sf = M["Sf"]
                    Sb, b_Sb = M["Sb"]
                    sg, b_sg = M["sg"]
                    rd, b_rd = M["rd"]
                    bwd_update(X, 1, c, True)
                    if stop <= 3.2:
                        continue
                    for d in range(2):
                        ke, b_ke = M["ke"][d]
                        qe, b_qe = M["qe"][d]

                        sbi = 4 if d == 0 else 7

                        def scm(e, ke=ke, qe=qe, X=X, cs=cs, sbi=sbi):
                            ins = None
                            for h in range(4):
                                if X == "A":
                                    ins = e.matmul(banks[sbi][:, h * 128:(h + 1) * 128], lhsT=ke[:, h, cs], rhs=qe[:, h, cs],
                                                   start=True, stop=True)
                                else:
                                    ins = e.matmul(banks[sbi][:, h * 128:(h + 1) * 128], lhsT=ke[:, h // 2, cs],
                                                   rhs=qe[:, h % 2, h // 2, cs], start=True, stop=True)
                            return ins
                        S.op("pe", scm, r=[b_ke, b_qe], w=[PB[sbi]])
                        dst, b_dst, msk, b_msk = (scF, b_scF, maskF, b_maskF) if d == 0 else (scBt, b_scBt, maskB, b_maskB)
                        S.op("dve", lambda e, dst=dst, msk=msk, sbi=sbi: e.tensor_tensor(
                            out=dst, in0=banks[sbi][:, :].rearrange("p (h t) -> p h t", h=4), in1=msk, op=ALU.mult),
                            r=[PB[sbi], b_msk], w=[b_dst])
                    if stop <= 3.4:
                        continue
                    qf, b_qf = M["qe"][0]
                    qb_, b_qb = M["qe"][1]

                    def om(e, X=X, v=v, Sf=Sf, Sb=Sb, qf=qf, qb_=qb_, c=c, cs=cs, cl=cl):
                        ins = None
                        for h in range(4):
                            o = banks[5][:, h * 128:(h + 1) * 128]
                            vv = v[:, c, h * 128:(h + 1) * 128]
                            e.matmul(o, lhsT=vv, rhs=scF[:, h, :], start=True, stop=False)
                            e.matmul(o, lhsT=vv, rhs=scBt[:, h, :], start=False, stop=False)
                            if X == "A":
                                e.matmul(o, lhsT=Sf[:, cl, h, :], rhs=qf[:, h, cs], start=False, stop=False)
                                ins = e.matmul(o, lhsT=Sb[:, h, :], rhs=qb_[:, h, cs], start=False, stop=True)
                            else:
                                kt = h // 2
                                e.matmul(o, lhsT=Sf[:, cl, kt, :], rhs=qf[:, h % 2, kt, cs],
                                         start=False, stop=False)
                                ins = e.matmul(o, lhsT=Sb[:, kt, :], rhs=qb_[:, h % 2, kt, cs],
                                               start=False, stop=True)
                        return ins
                    S.op("pe", om, r=[b_v, b_scF, b_scBt, b_Sf, b_Sb, b_qf, b_qb], w=[PB[5]])
                    if stop <= 3.6:
                        continue
                    o5 = banks[5][:, :].rearrange("p (h t) -> p h t", h=4)
                    S.op("dve", lambda e: e.tensor_copy(out=oTs, in_=o5), r=[PB[5]], w=[b_oTs])
                    S.op("act", lambda e: e.activation(out=sq, in_=oTs, func=AF.Square), r=[b_oTs], w=[b_sq])
                    if X == "A" and c == 1:
                        dump("oT_%d_%d" % (seq, sti), oTs, [b_oTs])
                    S.op("pe", lambda e: e.matmul(banks[7][:, :], lhsT=onesb, rhs=sq.rearrange("p h t -> p (h t)"),
                                                  start=True, stop=True), r=[b_onesb, b_sq], w=[PB[7]])
                    if stop <= 3.7:
                        continue
                    r2 = rstd.rearrange("p h t -> p (h t)")
                    S.op("act", lambda e: e.activation(out=r2, in_=banks[7][:, :], func=AF.Ln, scale=1.0 / 128.0,
                                                       bias=EPS), r=[PB[7]], w=[b_rstd])
                    S.op("act", lambda e: e.activation(out=r2, in_=r2, func=AF.Exp, scale=-0.5), r=[b_rstd], w=[b_rstd])
                    if stop <= 3.8:
                        continue
                    S.op("dve", lambda e: e.tensor_tensor(out=oTs, in0=oTs, in1=rstd, op=ALU.mult),
                         r=[b_oTs, b_rstd], w=[b_oTs])
                    nwc = M["nw"]
                    S.op("dve", lambda e, rd=rd, sg=sg, nwc=nwc, cs=cs: e.scalar_tensor_tensor(
                        out=rd[:, :, cs], in0=oTs, scalar=vecs[:, nwc:nwc + 1], in1=sg[:, :, cs],
                        op0=ALU.mult, op1=ALU.mult), r=[b_oTs, b_vecs, b_sg], w=[b_rd])
            dump("rdA_%d_%d" % (seq, sti), rdA, [b_rdA])
            dump("rdB_%d_%d" % (seq, sti), rdB, [b_rdB])
            if stop <= 4:
                return
            v4 = lambda w: w.rearrange("p k n -> p (k n)").rearrange("p (h n) -> p h n", h=4)
            wua, b_wua = wload(wupob_d[0:512, :].rearrange("(h p) n -> p h n", p=128), view=v4)
            wub, b_wub = wload(wupob_d[512:1024, :].rearrange("(h p) n -> p h n", p=128), view=v4)
            wua = v4(wua)
            wub = v4(wub)
            for m in range(8):
                bk, bb = pjbank()

                def um(e, m=m, bk=bk):
                    ins = None
                    for h in range(4):
                        ins = e.matmul(bk[:, 0:ST], lhsT=wua[:, h, m * 128:(m + 1) * 128], rhs=rdA[:, h, :],
                                       start=(h == 0), stop=(h == 3))
                    for h in range(4):
                        ins = e.matmul(bk[:, ST:2 * ST], lhsT=wub[:, h, m * 128:(m + 1) * 128], rhs=rdB[:, h, :],
                                       start=(h == 0), stop=(h == 3))
                    return ins
                S.op("pe", um, r=[b_wua, b_wub, b_rdA, b_rdB], w=[bb])
                S.op("dve", lambda e, m=m, bk=bk: e.tensor_tensor(out=mt1, in0=bk[:, 0:ST], in1=smA[:, m, :], op=ALU.mult),
                     r=[bb, b_smA], w=[b_mt1])
                S.op("dve", lambda e, m=m, bk=bk: e.tensor_tensor(out=mt2, in0=bk[:, ST:2 * ST], in1=smB[:, m, :],
                                                                 op=ALU.mult), r=[bb, b_smB], w=[b_mt2])
                S.op("pool", lambda e, m=m: e.tensor_tensor(out=mrgT[:, m, :], in0=mt1, in1=mt2, op=ALU.add),
                     r=[b_mt1, b_mt2], w=[b_mrgT])
            dump("mrgT_%d_%d" % (seq, sti), mrgT, [b_mrgT])
            if stop <= 5:
                return
            wo = [wload(wupob_d[1024:2048, n * 512:(n + 1) * 512].rearrange("(k p) n -> p k n", p=128)) for n in range(2)]
            for t in range(2):
                for n in range(2):
                    bk, bb = pjbank()
                    won, b_won = wo[n]

                    def omm(e, t=t, bk=bk, won=won):
                        ins = None
                        for kc in range(8):
                            ins = e.matmul(bk[:, :], lhsT=mrgT[:, kc, t * 128:(t + 1) * 128], rhs=won[:, kc, :],
                                           start=(kc == 0), stop=(kc == 7))
                        return ins
                    S.op("pe", omm, r=[b_mrgT, b_won], w=[bb])
                    S.op("act", lambda e, n=n, bk=bk: e.activation(out=mixs[:, n * 512:(n + 1) * 512], in_=bk[:, :],
                                                                    func=AF.Copy), r=[bb], w=[b_mixs])
                r0 = seq * LAT + sti * ST + t * 128
                S.op("sp", lambda e, r0=r0: e.dma_start(out=xres, in_=x_d[r0:r0 + 128, :]), w=[b_xres], dma=True)
                S.op("act", lambda e: e.activation(out=junk, in_=mixs, func=AF.Square, accum_out=st2[:, 4:5]),
                     r=[b_mixs], w=[b_junk, b_st2r])
                S.op("act", lambda e: e.activation(out=st2[:, 0:1], in_=st2[:, 4:5], func=AF.Copy),
                     r=[b_st2r], w=[b_st2])
                S.op("pool", lambda e: e.tensor_scalar(out=st2[:, 0:1], in0=st2[:, 0:1], scalar1=1.0 / D, scalar2=EPS,
                                                       op0=ALU.mult, op1=ALU.add), r=[b_st2], w=[b_st2])
                S.op("pool", lambda e: e.tensor_tensor(out=st2[:, 0:1], in0=st2[:, 0:1], in1=neghalf[:, 0:1], op=ALU.pow),
                     r=[b_st2, b_neghalf], w=[b_st2])
                S.op("dve", lambda e: e.tensor_tensor(out=mixs, in0=mixs, in1=G1, op=ALU.mult),
                     r=[b_mixs, b_G1], w=[b_mixs])
                S.op("dve", lambda e: e.scalar_tensor_tensor(out=x1t, in0=mixs, scalar=st2[:, 0:1], in1=xres,
                                                             op0=ALU.mult, op1=ALU.add),
                     r=[b_mixs, b_st2, b_xres], w=[b_x1t])
                S.op("sp", lambda e, r0=r0: e.dma_start(out=x1_d[r0:r0 + 128, :], in_=x1t), r=[b_x1t], w=[b_x1d],
                     dma=True)
                if dbg.get("moe", True):
                    m1_tile(r0 // 128)

        b_x1d = Buf("x1d")
        nseq = dbg.get("nseq", NSEQ)
        p2list = dbg.get("p2list", list(range(7, -1, -1)))
        sts = []
        for seq in range(nseq):
            sts.append(("p1", seq, True, 0))
            for sti in range(dbg.get("p1n", 8)):
                sts.append(("p1", seq, False, sti))
            for sti in p2list:
                sts.append(("p2", seq, False, sti))

        def emit_load(desc):
            kind, seq, is_ctx, sti = desc
            if is_ctx:
                load_norm_transpose(ctx_d[seq * CTXL:(seq + 1) * CTXL, :], 2)
            else:
                load_norm_transpose(x_d[seq * LAT + sti * ST:seq * LAT + (sti + 1) * ST, :], seq)

        emit_load(sts[0])
        for i, desc in enumerate(sts):
            kind, seq, is_ctx, sti = desc
            if is_ctx:
                for X in ("A", "B"):
                    for key in ("W", "V"):
                        ap_, bf_ = MIX[X][key]
                        S.op("pool", lambda e, ap_=ap_: e.memset(ap_, 0.0), w=[bf_])
                S.op("sp", lambda e, seq=seq: e.dma_start(out=G1, in_=bc_d[seq, 0:1, :].to_broadcast([128, D])),
                     r=[b_bcd], w=[b_G1], dma=True)
                S.op("sp", lambda e, seq=seq: e.dma_start(out=SH2, in_=bc_d[seq, 1:2, :].to_broadcast([128, D])),
                     r=[b_bcd], w=[b_SH2], dma=True)
                S.op("sp", lambda e, seq=seq: e.dma_start(out=A2, in_=bc_d[seq, 2:3, :].to_broadcast([128, D])),
                     r=[b_bcd], w=[b_A2], dma=True)
            if i + 1 < len(sts):
                pf["fn"] = (lambda d_=sts[i + 1]: emit_load(d_))
            if kind == "p1":
                pass1_st(seq, is_ctx, sti)
                if is_ctx and seq == 0:
                    dump("WA_ctx", WA, [b_WA])
                    dump("VA_ctx", VA, [b_VA])
                    dump("WB_ctx", WB, [b_WB])
                    dump("VB_ctx", VB, [b_VB])
            else:
                pass2_st(seq, sti)
            run_prefetch()
        if "x1" in dbg_out:
            xr0, xr1 = dbg.get("x1rows", (0, NSEQ * LAT))
            S.op("sp", lambda e: e.dma_start(out=dbg_out["x1"], in_=x1_d[xr0:xr1, :]), r=[b_x1d], w=[Buf("o")], dma=True)

        if dbg.get("moe", True) and dbg.get("m2", True):
            A.release(mix_mark)
            S.fence()
            wts = [[T([8, 1024], BF16, "w%s%d" % (nm, i)) for nm in ("g", "u", "d")] for i in range(2)]
            xgt = [T([2, 1024], BF16, "xgt%d" % i) for i in range(2)]
            xgTs = [T([8, GR], BF16, "xgT%d" % i) for i in range(2)]
            xgT1bufs = [Buf("xgT1_%d" % i) for i in range(2)]
            actTs = [T([8, GR], BF16, "actT%d" % i) for i in range(2)]
            rrb = [T([GR], F32, "rr%d" % i) for i in range(2)]
            sgb = [T([GR], F32, "sg%d" % i) for i in range(2)]
            u1b = [T([GR], F32, "u1%d" % i) for i in range(2)]
            t1b = [T([GR], F32, "t1%d" % i) for i in range(2)]
            ytl = [T([1024], F32, "yt%d" % i) for i in range(2)]
            bg7, b_bg7 = T([256], F32, "bg7")
            S.op("dve", lambda e: e.tensor_scalar(out=bg7, in0=vecs[:, V_BG:V_BG + 256], scalar1=-1.0, scalar2=7.0,
                                                  op0=ALU.mult, op1=ALU.add), r=[b_vecs], w=[b_bg7])
            elist = dbg.get("elist", list(range(NEXP)))
            ngr = dbg.get("ngr", NGR)
            use_guard = dbg.get("guard", True)
            S.gran = GR
            S.op("dve", lambda e: e.tensor_copy(out=cnti, in_=basec), r=[b_basec], w=[b_cnti])
            gctr = [0]
            guc = [0]
            dnc = [0]
            for ei, ex in enumerate(elist):
                (wg, b_wg), (wu, b_wu), (wd, b_wd) = wts[ei % 2]
                for (wt_, wb__, src) in ((wg, b_wg, wg_d), (wu, b_wu, wu_d), (wd, b_wd, wd_d)):
                    S.op("pool", lambda e, wt_=wt_, src=src, ex=ex: e.dma_start(
                        out=wt_, in_=src[ex].rearrange("(k p) n -> p k n", p=128)), w=[wb__], dma=True)
                if use_guard:
                    for en in ("pe", "act", "dve", "sp"):
                        def ldc(e, ex=ex, en=en):
                            if en not in S.cnt_regs:
                                S.cnt_regs[en] = e.alloc_register("cnt_" + en)
                            e.load(S.cnt_regs[en], cnti[0:1, ex:ex + 1])
                            return e.drain()
                        S.op(en, ldc, r=[b_cnti])
                def make_granule(ex, g, wg, wu, wd, b_wg, b_wu, b_wd):
                    r0 = ex * CAP + g * GR
                    par = gctr[0] % 2
                    gctr[0] += 1
                    xa, xb_ = xgt[par]
                    xgT, b_xgT = xgTs[par]
                    b_xgT1 = xgT1bufs[par]
                    actT, b_actT = actTs[par]

                    def part_load_tr(GD):
                        S.op("sp", lambda e, xa=xa, r0=r0: e.dma_start(
                            out=xa, in_=xg_d[r0:r0 + GR, :].rearrange("(t p) n -> p t n", p=128)),
                            r=[b_xg], w=[xb_], dma=True, guard=GD)
                        for hf in range(2):
                            tb = 4 + hf
                            tpv = banks[tb][:, :].bitcast(BF16).rearrange("p (k n) -> p k n", k=4)

                            def tr(e, xa=xa, hf=hf, tpv=tpv):
                                ins = None
                                for q in range(4):
                                    kc = hf * 4 + q
                                    for t in range(2):
                                        ins = e.transpose(out=tpv[:, q, t * 128:(t + 1) * 128],
                                                          in_=xa[:, t, kc * 128:(kc + 1) * 128], identity=identb)
                                return ins
                            S.op("pe", tr, r=[xb_, b_identb], w=[PB[tb]], guard=GD)
                            if hf == 0:
                                S.op("act", lambda e, hf=hf, tpv=tpv, xgT=xgT: e.activation(
                                    out=xgT[:, hf * 4:(hf + 1) * 4, :], in_=tpv, func=AF.Copy), r=[PB[tb]], w=[b_xgT], guard=GD)
                            else:
                                S.op("dve", lambda e, hf=hf, tpv=tpv, xgT=xgT: e.tensor_copy(
                                    out=xgT[:, hf * 4:(hf + 1) * 4, :], in_=tpv), r=[PB[tb]], w=[b_xgT1], guard=GD)

                    def part_fc(GD):
                        for fc in range(8):
                            bi = guc[0] % 2
                            guc[0] += 1
                            bk, bb = banks[bi], PB[bi]
                            bku, bbu = banks[2 + bi], PB[2 + bi]

                            def gmm(e, fc=fc, bk=bk, wg=wg, xgT=xgT):
                                ins = None
                                for kc in range(8):
                                    ins = e.matmul(bk[:, 0:GR], lhsT=wg[:, kc, fc * 128:(fc + 1) * 128], rhs=xgT[:, kc, :],
                                                   start=(kc == 0), stop=(kc == 7))
                                return ins

                            def umm(e, fc=fc, bku=bku, wu=wu, xgT=xgT):
                                ins = None
                                for kc in range(8):
                                    ins = e.matmul(bku[:, 0:GR], lhsT=wu[:, kc, fc * 128:(fc + 1) * 128], rhs=xgT[:, kc, :],
                                                   start=(kc == 0), stop=(kc == 7))
                                return ins
                            S.op("pe", gmm, r=[b_wg, b_xgT, b_xgT1], w=[bb], guard=GD)
                            S.op("pe", umm, r=[b_wu, b_xgT, b_xgT1], w=[bbu], guard=GD)
                            rr, b_rr = rrb[fc % 2]
                            sg_, b_sg_ = sgb[fc % 2]
                            u1, b_u1 = u1b[fc % 2]
                            t1, b_t1 = t1b[fc % 2]
                            cb = ex * 8 + fc
                            cu = V_BU + ex * 8 + fc
                            S.op("act", lambda e, bk=bk, rr=rr, cb=cb: e.activation(
                                out=rr, in_=bk[:, 0:GR], func=AF.Relu, scale=-1.0, bias=bg7[:, cb:cb + 1]),
                                r=[bb, b_bg7], w=[b_rr], guard=GD)
                            S.op("dve", lambda e, bku=bku, u1=u1, cu=cu: e.tensor_scalar(
                                out=u1, in0=bku[:, 0:GR], scalar1=vecs[:, cu:cu + 1], scalar2=7.0, op0=ALU.add, op1=ALU.min),
                                r=[bbu, b_vecs], w=[b_u1], guard=GD)
                            S.op("act", lambda e, rr=rr, sg_=sg_: e.activation(out=sg_, in_=rr, func=AF.Sigmoid, scale=-1.702,
                                                                               bias=1.702 * 7.0), r=[b_rr], w=[b_sg_], guard=GD)
                            S.op("dve", lambda e, u1=u1: e.tensor_scalar(out=u1, in0=u1, scalar1=-7.0, scalar2=1.0,
                                                                        op0=ALU.max, op1=ALU.add), r=[b_u1], w=[b_u1], guard=GD)
                            S.op("dve", lambda e, rr=rr, sg_=sg_, t1=t1: e.scalar_tensor_tensor(
                                out=t1, in0=rr, scalar=7.0, in1=sg_, op0=ALU.subtract, op1=ALU.mult),
                                r=[b_rr, b_sg_], w=[b_t1], guard=GD)
                            S.op("dve", lambda e, t1=t1, u1=u1, fc=fc, actT=actT: e.tensor_tensor(
                                out=actT[:, fc, :], in0=t1, in1=u1, op=ALU.mult), r=[b_t1, b_u1], w=[b_actT], guard=GD)

                    def part_dn(GD):
                        for t in range(2):
                            ya, yb_ = ytl[t]
                            for n in range(2):
                                bi = 6 + dnc[0] % 2
                                dnc[0] += 1
                                bk, bb = banks[bi], PB[bi]

                                def dn(e, t=t, n=n, bk=bk, wd=wd, actT=actT):
                                    ins = None
                                    for fc in range(8):
                                        ins = e.matmul(bk[:, :], lhsT=actT[:, fc, t * 128:(t + 1) * 128],
                                                       rhs=wd[:, fc, n * 512:(n + 1) * 512], start=(fc == 0), stop=(fc == 7))
                                    return ins
                                S.op("pe", dn, r=[b_actT, b_wd], w=[bb], guard=GD)
                                S.op("act", lambda e, ya=ya, n=n, bk=bk: e.activation(out=ya[:, n * 512:(n + 1) * 512],
                                                                                     in_=bk[:, :], func=AF.Copy, scale=-1.0),
                                     r=[bb], w=[yb_], guard=GD)
                            S.op("sp", lambda e, ya=ya, r0=r0, t=t: e.dma_start(out=yg_d[r0 + t * 128:r0 + (t + 1) * 128, :],
                                                                                in_=ya), r=[yb_], w=[b_yg], dma=True, guard=GD)

                    return part_load_tr, part_fc, part_dn

                grs = [make_granule(ex, g, wg, wu, wd, b_wg, b_wu, b_wd) for g in range(ngr)]
                gdf = (lambda g_, ex=ex: (ex, g_)) if use_guard else (lambda g_: None)
                grs[0][0](gdf(0))
                for g in range(ngr):
                    grs[g][1](gdf(g))
                    if g + 1 < ngr:
                        grs[g + 1][0](gdf(g))
                    grs[g][2](gdf(g))
            A.release(mix_mark)
            S.fence()
            yks = [[T([1024], F32, "yk%d_%d" % (k, i)) for k in range(4)] for i in range(2)]
            accs = [T([1024], F32, "acc%d" % i) for i in range(2)]
            x1rs = [T([1024], F32, "x1r%d" % i) for i in range(2)]
            PTs = [T([128], F32, "PT%d" % i) for i in range(2)]
            sqj, b_sqj = T([1024], BF16, "sqj")
            G2, b_G2 = T([1024], F32, "G2")
            bdn, b_bdn = T([1024], F32, "bdn")
            st3, b_st3 = T([8], F32, "st3")
            b_st3r = Buf("st3r")
            S.op("sp", lambda e: e.dma_start(out=bdn[0:NEXP], in_=bd_d), w=[b_bdn], dma=True)
            tlist = dbg.get("m3tiles", list(range(NTT)))
            curseq = [-1]
            def m3_bufs(ti):
                return yks[ti % 2], accs[ti % 2], x1rs[ti % 2], PTs[ti % 2]

            def m3_gather(ti, gt):
                yk, (acc, b_acc), (x1r, b_x1r), (PT, b_PT) = m3_bufs(ti)
                seq = gt // (LAT // 128)
                for k in range(4):
                    S.op("pool", lambda e, k=k, gt=gt, yk=yk: e.indirect_dma_start(
                        out=yk[k][0][:, :], out_offset=None, in_=yg_d[:, :],
                        in_offset=bass.IndirectOffsetOnAxis(ap=desti[:, gt, k:k + 1], axis=0),
                        bounds_check=bcreg(e), oob_is_err=False),
                        r=[b_yg, b_desti], w=[yk[k][1]], dma=True)
                S.op("sp", lambda e, gt=gt, x1r=x1r: e.dma_start(out=x1r, in_=x1_d[gt * 128:(gt + 1) * 128, :]),
                     r=[b_x1d], w=[b_x1r], dma=True)

            def m3_compute(ti, gt):
                yk, (acc, b_acc), (x1r, b_x1r), (PT, b_PT) = m3_bufs(ti)
                seq = gt // (LAT // 128)
                if seq != curseq[0]:
                    curseq[0] = seq
                    S.op("sp", lambda e, seq=seq: e.dma_start(out=G2, in_=bc_d[seq, 3:4, :].to_broadcast([128, D])),
                         r=[b_bcd], w=[b_G2], dma=True)
                bk, bb = pjbank()
                S.op("pe", lambda e, bk=bk, gt=gt: e.transpose(out=bk[0:NEXP, 0:128], in_=P_all[:, gt, :], identity=identf),
                     r=[b_Pall, b_identf], w=[bb])
                S.op("act", lambda e, bk=bk, PT=PT: e.activation(out=PT[0:NEXP], in_=bk[0:NEXP, 0:128], func=AF.Copy),
                     r=[bb], w=[b_PT])
                bks = []
                for n in range(2):
                    bk2, bb2 = pjbank()
                    S.op("pe", lambda e, bk2=bk2, n=n, PT=PT: e.matmul(bk2[:, :], lhsT=PT[0:NEXP], rhs=bdn[0:NEXP, n * 512:(n + 1) * 512],
                                                               start=True, stop=True), r=[b_PT, b_bdn], w=[bb2])
                    bks.append((bk2, bb2))
                S.op("dve", lambda e, gt=gt, acc=acc, yk=yk: e.tensor_scalar(out=acc, in0=yk[0][0], scalar1=pk_all[:, gt, 0:1], scalar2=None,
                                                             op0=ALU.mult), r=[yk[0][1], b_pkall], w=[b_acc])
                for k in range(1, 4):
                    S.op("dve", lambda e, k=k, gt=gt, acc=acc, yk=yk: e.scalar_tensor_tensor(out=acc, in0=yk[k][0], scalar=pk_all[:, gt, k:k + 1],
                                                                             in1=acc, op0=ALU.mult, op1=ALU.add),
                         r=[yk[k][1], b_pkall, b_acc], w=[b_acc])
                for n in range(2):
                    bk2, bb2 = bks[n]
                    S.op("dve", lambda e, bk2=bk2, n=n, acc=acc: e.tensor_tensor(out=acc[:, n * 512:(n + 1) * 512], in0=bk2[:, :],
                                                                        in1=acc[:, n * 512:(n + 1) * 512], op=ALU.add),
                         r=[bb2, b_acc], w=[b_acc])
                dump("ffn_%d" % gt, acc, [b_acc])

                S.op("act", lambda e, acc=acc: e.activation(out=sqj, in_=acc, func=AF.Square, accum_out=st3[:, 4:5]),
                     r=[b_acc], w=[b_sqj, b_st3r])
                S.op("act", lambda e: e.activation(out=st3[:, 0:1], in_=st3[:, 4:5], func=AF.Copy),
                     r=[b_st3r], w=[b_st3])
                S.op("pool", lambda e: e.tensor_scalar(out=st3[:, 0:1], in0=st3[:, 0:1], scalar1=1.0 / D, scalar2=EPS,
                                                       op0=ALU.mult, op1=ALU.add), r=[b_st3], w=[b_st3])
                S.op("pool", lambda e: e.tensor_tensor(out=st3[:, 0:1], in0=st3[:, 0:1], in1=neghalf[:, 0:1], op=ALU.pow),
                     r=[b_st3, b_neghalf], w=[b_st3])
                S.op("dve", lambda e, acc=acc: e.tensor_tensor(out=acc, in0=acc, in1=G2, op=ALU.mult), r=[b_acc, b_G2], w=[b_acc])
                S.op("dve", lambda e, acc=acc, x1r=x1r: e.scalar_tensor_tensor(out=acc, in0=acc, scalar=st3[:, 0:1], in1=x1r,
                                                             op0=ALU.mult, op1=ALU.add),
                     r=[b_acc, b_st3, b_x1r], w=[b_acc])
                S.op("sp", lambda e, gt=gt, acc=acc: e.dma_start(out=out_d[gt * 128:(gt + 1) * 128, :], in_=acc),
                     r=[b_acc], w=[Buf("outd")], dma=True)


            if tlist:
                m3_gather(0, tlist[0])
            for ti, gt in enumerate(tlist):
                if ti + 1 < len(tlist):
                    m3_gather(ti + 1, tlist[ti + 1])
                m3_compute(ti, gt)

        dump("P_all", P_all, [b_Pall])
        dump("pk_all", pk_all, [b_pkall])
        dump("desti", desti, [b_desti])
        print("arena high water", A.hi)

        fin = Buf("fin")
        last = []
        for e in ENGS:
            if S.ops[e]:
                last.append(S.ops[e][-1])
        alld = [o for e in ENGS for o in S.ops[e] if o.dma]
        fo = S.op("sp", lambda e: e.nop(), w=[fin])
        fo.deps.update(alld)
        fo.deps.update(last)
        fo.deps.discard(fo)

        S.finalize(nc, ctx)
        with nc.Block() as block:
            @block.tensor
            def _(e):
                S.emit("pe", e)

            @block.scalar
            def _(e):
                S.emit("act", e)

            @block.vector
            def _(e):
                S.emit("dve", e)

            @block.gpsimd
            def _(e):
                S.emit("pool", e)

            @block.sync
            def _(e):
                S.emit("sp", e)
    return nc


def _fm(v, n):
    return np.ascontiguousarray(np.asarray(v, np.float32).reshape(n, 128).T)


def make_core_inputs(core, inp):
    b0 = core * NSEQ
    x = np.ascontiguousarray(inp["x"][b0:b0 + NSEQ].reshape(NSEQ * LAT, D))
    cx = np.ascontiguousarray(inp["ctx"][b0:b0 + NSEQ].reshape(NSEQ * CTXL, D))
    cs = np.stack([inp["c"][b0], inp["c"][b0 + 1], inp["c_ctx"]], axis=-1)
    cT = np.ascontiguousarray(cs.reshape(8, 128, 3).transpose(1, 0, 2))
    vecs = np.zeros((128, NV), np.float32)
    vecs[:, V_BADA:V_BADA + 48] = _fm(inp["b_ada"][0], 48)
    vecs[:, V_GPRE:V_GPRE + 8] = _fm(inp["g_pre_mix"][0], 8)
    lb = inp["hgrn_lb"]
    for s in range(2):
        for d in range(2):
            vecs[:, V_LBR + s * 8 + d * 4:V_LBR + s * 8 + d * 4 + 4] = _fm(lb[s, d], 4)
    vecs[:, V_HNW] = inp["hgrn_norm_w"][0]
    vecs[:, V_GNW] = inp["gla_norm_w"][0]
    for d in range(2):
        vecs[:, V_GKB + d * 2:V_GKB + d * 2 + 2] = _fm(inp["gla_gk_b"][0, d], 2)
    vecs[:, V_BG:V_BG + 256] = np.asarray(inp["b_gate"][0]).reshape(32, 8, 128).transpose(2, 0, 1).reshape(128, 256)
    vecs[:, V_BU:V_BU + 256] = np.asarray(inp["b_up"][0]).reshape(32, 8, 128).transpose(2, 0, 1).reshape(128, 256)
    rowv = np.zeros((8, D), np.float32)
    ba = inp["b_ada"][0]
    rowv[0] = ba[2048:3072]
    rowv[1] = ba[3072:4096]
    rowv[2] = ba[4096:5120]
    rowv[3] = ba[5120:6144]
    rowv[4] = inp["g_post_mix"][0]
    rowv[5] = inp["g_pre_ffn"][0]
    rowv[6] = inp["g_post_ffn"][0]
    rowv[7, :32] = inp["b_router"][0]
    wupo = np.concatenate([inp["w_up_a"][0], inp["w_up_b"][0], inp["w_o"][0]], axis=0)
    return {
        "x": x, "ctx": cx, "cT": cT, "vecs": vecs, "rowv": rowv,
        "w_ada": np.ascontiguousarray(inp["w_ada"][0]), "w_in": np.ascontiguousarray(inp["w_in"][0]),
        "gk_w2": np.ascontiguousarray(inp["gla_gk_w2"][0]), "wupo": np.ascontiguousarray(wupo),
        "w_router": np.ascontiguousarray(inp["w_router"][0]),
        "w_gate": np.ascontiguousarray(inp["w_gate"][0]), "w_up": np.ascontiguousarray(inp["w_up"][0]),
        "w_down": np.ascontiguousarray(inp["w_down"][0]), "b_down": np.ascontiguousarray(inp["b_down"][0]),
    }


def kernel(**inputs):
    inp = {k: np.asarray(v) for k, v in inputs.items()}
    nc = build_program()
    in_maps = [make_core_inputs(c, inp) for c in range(8)]
    res = run_bass_kernel_spmd(nc, in_maps, core_ids=list(range(8)))
    outs = [np.asarray(r["out"]).reshape(NSEQ, LAT, D) for r in res.results]
    return np.concatenate(outs, axis=0).astype(np.float32)
```
